# Optimizing a Trainium2 kernel written in Bass

```python
import math
import jax, jax.numpy as jnp
from jax import lax
import numpy as np

D_MODEL = 1024
BATCH = 4
SEQ = 4096
DEPTH = 1

DA_HEADS = 4
DA_QK_DIM = 64
DA_V_DIM = 2 * DA_QK_DIM
DA_WIDTH = DA_HEADS * DA_V_DIM
HG_HEADS = 4
HG_DK = 128
HG_DV = 128
HG_WIDTH = HG_HEADS * HG_DV
MIX_WIDTH = DA_WIDTH + HG_WIDTH
DA_Q_COLS = DA_HEADS * 2 * DA_QK_DIM
DA_K_COLS = DA_HEADS * 2 * DA_QK_DIM
DA_V_COLS = DA_WIDTH
HG_Q_COLS = HG_HEADS * HG_DK
HG_F_COLS = HG_HEADS * HG_DK
HG_I_COLS = HG_WIDTH
HG_G_COLS = HG_WIDTH
IN_WIDTH = DA_Q_COLS + DA_K_COLS + DA_V_COLS + HG_Q_COLS + HG_F_COLS + HG_I_COLS + HG_G_COLS
IN_SPLITS = (DA_Q_COLS,
             DA_Q_COLS + DA_K_COLS,
             DA_Q_COLS + DA_K_COLS + DA_V_COLS,
             DA_Q_COLS + DA_K_COLS + DA_V_COLS + HG_Q_COLS,
             DA_Q_COLS + DA_K_COLS + DA_V_COLS + HG_Q_COLS + HG_F_COLS,
             DA_Q_COLS + DA_K_COLS + DA_V_COLS + HG_Q_COLS + HG_F_COLS + HG_I_COLS)
ROPE_THETA = 500000.0
ROT_DIM = DA_QK_DIM // 4
Q_BLOCK = 128
HG_CHUNK = 64
N_GROUPS = 4
EXPERTS_PER_GROUP = 8
N_EXPERTS = N_GROUPS * EXPERTS_PER_GROUP
TOP_K = 2
EXPERT_FF = 256
MOE_BLOCK = 128
EPS = 1e-6

kernel_name = "hymba_diffattn_hgrn2_hmoe_adaln"


def rmsnorm(x, w):
    xf = x.astype(jnp.float32)
    y = xf * lax.rsqrt(jnp.mean(xf * xf, axis=-1, keepdims=True) + EPS)
    return (y * w.astype(jnp.float32)).astype(x.dtype)


def rope_partial(t, positions):
    half = ROT_DIM // 2
    inv_freq = ROPE_THETA ** (-jnp.arange(half, dtype=jnp.float32) / half)
    ang = positions.astype(jnp.float32)[..., None] * inv_freq
    cos = jnp.cos(ang)[:, :, None, :]
    sin = jnp.sin(ang)[:, :, None, :]
    tr = t[..., :ROT_DIM].astype(jnp.float32)
    t1, t2 = tr[..., :half], tr[..., half:]
    rot = jnp.concatenate([t1 * cos - t2 * sin, t2 * cos + t1 * sin], axis=-1).astype(t.dtype)
    return jnp.concatenate([rot, t[..., ROT_DIM:]], axis=-1)


def diff_attention(q, k, v, lam_q1, lam_k1, lam_q2, lam_k2, subln_w, layer_idx):
    B, S, H, _, Dk = q.shape
    lam_init = 0.8 - 0.6 * math.exp(-0.3 * layer_idx)
    lam = (jnp.exp(jnp.sum(lam_q1.astype(jnp.float32) * lam_k1.astype(jnp.float32)))
           - jnp.exp(jnp.sum(lam_q2.astype(jnp.float32) * lam_k2.astype(jnp.float32)))
           + lam_init)
    scale = Dk ** -0.5
    nb = S // Q_BLOCK
    q_blocks = q.reshape(B, nb, Q_BLOCK, H, 2, Dk).transpose(1, 0, 2, 3, 4, 5)
    key_idx = jnp.arange(S)

    def one_block(args):
        qb, bi = args
        s = jnp.einsum('bqhmd,bkhmd->bhmqk', qb, k,
                       preferred_element_type=jnp.float32) * scale
        q_idx = bi * Q_BLOCK + jnp.arange(Q_BLOCK)
        causal = key_idx[None, :] <= q_idx[:, None]
        p = jax.nn.softmax(jnp.where(causal, s, -jnp.inf), axis=-1)
        a = p[:, :, 0] - lam * p[:, :, 1]
        return jnp.einsum('bhqk,bkhd->bqhd', a.astype(v.dtype), v)

    out = lax.map(one_block, (q_blocks, jnp.arange(nb)))
    out = out.transpose(1, 0, 2, 3, 4).reshape(B, S, H, v.shape[-1])
    return rmsnorm(out, subln_w) * (1.0 - lam_init)


def hgrn2_chunkwise(q, f_raw, i, g, lb, norm_w):
    B, S, H, DK = q.shape
    DV = i.shape[-1]
    nc = S // HG_CHUNK
    f32 = jnp.float32
    qf = jax.nn.silu(q.astype(f32))
    f = lb + (1.0 - lb) * jax.nn.sigmoid(f_raw.astype(f32))
    log_f = jnp.log(f)
    kf = 1.0 - f

    def to_chunks(t):
        return t.reshape(B, nc, HG_CHUNK, H, t.shape[-1]).transpose(1, 0, 3, 2, 4)

    tri = jnp.arange(HG_CHUNK)[:, None] >= jnp.arange(HG_CHUNK)[None, :]

    def step(state, inp):
        qc, kc, vc, lfc = inp
        b = jnp.cumsum(lfc, axis=2)
        b_last = b[:, :, -1:, :]
        o_inter = jnp.einsum('bhtk,bhkv->bhtv', qc * jnp.exp(b), state)
        rel = jnp.where(tri[:, :, None], b[:, :, :, None, :] - b[:, :, None, :, :], -jnp.inf)
        scores = jnp.einsum('bhtk,bhsk,bhtsk->bhts', qc, kc, jnp.exp(rel))
        o = o_inter + jnp.einsum('bhts,bhsv->bhtv', scores, vc)
        state = (jnp.exp(b_last[:, :, 0, :])[..., None] * state
                 + jnp.einsum('bhsk,bhsv->bhkv', kc * jnp.exp(b_last - b), vc))
        return state, o

    s0 = jnp.zeros((B, H, DK, DV), f32)
    _, o = lax.scan(step, s0, (to_chunks(qf), to_chunks(kf), to_chunks(i.astype(f32)), to_chunks(log_f)))
    o = o.transpose(1, 0, 3, 2, 4).reshape(B, S, H, DV)
    o = rmsnorm(o, norm_w) * jax.nn.silu(g.astype(f32))
    return o.astype(q.dtype)


def hier_moe(h, w_group, b_group, w_router, b_router, w_gate, w_up, w_down):
    B, S, D = h.shape
    T = B * S
    t = h.reshape(T, D)
    g_logits = jnp.einsum('td,dg->tg', t, w_group, preferred_element_type=jnp.float32) + b_group
    g_prob = jax.nn.softmax(g_logits, axis=-1)
    g_idx = jnp.argmax(g_logits, axis=-1)
    g_w = jnp.take_along_axis(g_prob, g_idx[:, None], axis=1)[:, 0]
    e_logits = (jnp.einsum('td,de->te', t, w_router, preferred_element_type=jnp.float32)
                + b_router).reshape(T, N_GROUPS, EXPERTS_PER_GROUP)
    e_logits = jnp.take_along_axis(e_logits, g_idx[:, None, None], axis=1)[:, 0]
    e_prob = jax.nn.softmax(e_logits, axis=-1)
    top_p, top_i = lax.top_k(e_prob, TOP_K)
    top_p = top_p / jnp.sum(top_p, axis=-1, keepdims=True)
    expert_ids = g_idx[:, None] * EXPERTS_PER_GROUP + top_i
    weights = top_p * g_w[:, None]
    combine = jnp.sum(jax.nn.one_hot(expert_ids, N_EXPERTS, dtype=jnp.float32)
                      * weights[..., None], axis=1)

    nb = T // MOE_BLOCK

    def one_block(args):
        tb, cb = args
        hg = jnp.einsum('td,edf->tef', tb, w_gate)
        hu = jnp.einsum('td,edf->tef', tb, w_up)
        act = jax.nn.silu(hg) * hu * cb[..., None].astype(tb.dtype)
        return jnp.einsum('tef,efd->td', act, w_down)

    y = lax.map(one_block, (t.reshape(nb, MOE_BLOCK, D), combine.reshape(nb, MOE_BLOCK, N_EXPERTS)))
    return y.reshape(B, S, D)


def setup_inputs(seed: int = 0) -> dict:
    key = jax.random.key(seed)
    ks = jax.random.split(key, 24)
    f32 = jnp.float32
    nrm = lambda k, shape, s: jax.random.normal(k, shape, f32) * s
    return {
        "x": nrm(ks[0], (BATCH, SEQ, D_MODEL), 1.0),
        "c": nrm(ks[1], (BATCH, D_MODEL), 1.0),
        "positions": jnp.broadcast_to(jnp.arange(SEQ, dtype=jnp.int32), (BATCH, SEQ)),
        "norm1_w": 1.0 + nrm(ks[2], (DEPTH, D_MODEL), 0.02),
        "norm2_w": 1.0 + nrm(ks[3], (DEPTH, D_MODEL), 0.02),
        "final_norm_w": 1.0 + nrm(ks[4], (D_MODEL,), 0.02),
        "ada_w": nrm(ks[5], (DEPTH, D_MODEL, 6 * D_MODEL), 0.5 * D_MODEL ** -0.5),
        "ada_b": nrm(ks[6], (DEPTH, 6 * D_MODEL), 0.02),
        "w_in": nrm(ks[7], (DEPTH, D_MODEL, IN_WIDTH), D_MODEL ** -0.5),
        "w_out": nrm(ks[8], (DEPTH, MIX_WIDTH, D_MODEL), MIX_WIDTH ** -0.5),
        "da_lambda_q1": nrm(ks[9], (DEPTH, DA_QK_DIM), 0.1),
        "da_lambda_k1": nrm(ks[10], (DEPTH, DA_QK_DIM), 0.1),
        "da_lambda_q2": nrm(ks[11], (DEPTH, DA_QK_DIM), 0.1),
        "da_lambda_k2": nrm(ks[12], (DEPTH, DA_QK_DIM), 0.1),
        "da_subln_w": 1.0 + nrm(ks[13], (DEPTH, DA_V_DIM), 0.02),
        "hg_lower_bound": nrm(ks[14], (DEPTH + 1, HG_HEADS * HG_DK), 0.1),
        "hg_norm_w": 1.0 + nrm(ks[15], (DEPTH, HG_DV), 0.02),
        "moe_w_group": nrm(ks[16], (DEPTH, D_MODEL, N_GROUPS), D_MODEL ** -0.5),
        "moe_b_group": nrm(ks[17], (DEPTH, N_GROUPS), 0.01),
        "moe_w_router": nrm(ks[18], (DEPTH, D_MODEL, N_EXPERTS), D_MODEL ** -0.5),
        "moe_b_router": nrm(ks[19], (DEPTH, N_EXPERTS), 0.01),
        "moe_w_gate": nrm(ks[20], (DEPTH, N_EXPERTS, D_MODEL, EXPERT_FF), D_MODEL ** -0.5),
        "moe_w_up": nrm(ks[21], (DEPTH, N_EXPERTS, D_MODEL, EXPERT_FF), D_MODEL ** -0.5),
        "moe_w_down": nrm(ks[22], (DEPTH, N_EXPERTS, EXPERT_FF, D_MODEL), EXPERT_FF ** -0.5),
    }


def reference(x, c, positions, norm1_w, norm2_w, final_norm_w, ada_w, ada_b, w_in, w_out,
              da_lambda_q1, da_lambda_k1, da_lambda_q2, da_lambda_k2, da_subln_w,
              hg_lower_bound, hg_norm_w, moe_w_group, moe_b_group, moe_w_router, moe_b_router,
              moe_w_gate, moe_w_up, moe_w_down):
    B, S, D = x.shape
    lower_bounds = jnp.cumsum(jax.nn.softmax(hg_lower_bound.astype(jnp.float32), axis=0), axis=0)
    c_act = jax.nn.silu(c)
    for l in range(DEPTH):
        mod = jnp.einsum('bd,de->be', c_act, ada_w[l]) + ada_b[l]
        shift1, scale1, gate1, shift2, scale2, gate2 = jnp.split(mod, 6, axis=-1)

        h = rmsnorm(x, norm1_w[l]) * (1.0 + scale1[:, None]) + shift1[:, None]
        proj = jnp.einsum('bsd,de->bse', h, w_in[l])
        dq, dk, dv, gq, gf, gi, gg = jnp.split(proj, IN_SPLITS, axis=-1)
        dq = rope_partial(dq.reshape(B, S, DA_HEADS * 2, DA_QK_DIM), positions)
        dk = rope_partial(dk.reshape(B, S, DA_HEADS * 2, DA_QK_DIM), positions)
        da_out = diff_attention(dq.reshape(B, S, DA_HEADS, 2, DA_QK_DIM),
                                dk.reshape(B, S, DA_HEADS, 2, DA_QK_DIM),
                                dv.reshape(B, S, DA_HEADS, DA_V_DIM),
                                da_lambda_q1[l], da_lambda_k1[l], da_lambda_q2[l], da_lambda_k2[l],
                                da_subln_w[l], l)
        hg_out = hgrn2_chunkwise(gq.reshape(B, S, HG_HEADS, HG_DK),
                                 gf.reshape(B, S, HG_HEADS, HG_DK),
                                 gi.reshape(B, S, HG_HEADS, HG_DV),
                                 gg.reshape(B, S, HG_HEADS, HG_DV),
                                 lower_bounds[l].reshape(HG_HEADS, HG_DK), hg_norm_w[l])
        mixed = jnp.concatenate([da_out.reshape(B, S, DA_WIDTH).astype(x.dtype),
                                 hg_out.reshape(B, S, HG_WIDTH).astype(x.dtype)], axis=-1)
        x = x + gate1[:, None] * jnp.einsum('bse,ed->bsd', mixed, w_out[l])

        h2 = rmsnorm(x, norm2_w[l]) * (1.0 + scale2[:, None]) + shift2[:, None]
        y = hier_moe(h2, moe_w_group[l], moe_b_group[l], moe_w_router[l], moe_b_router[l],
                     moe_w_gate[l], moe_w_up[l], moe_w_down[l])
        x = x + gate2[:, None] * y
    return rmsnorm(x, final_norm_w)
```

```python
import numpy as np
import ml_dtypes
import concourse.bass as bass
import concourse.mybir as mybir
from concourse.bass_utils import run_bass_kernel_spmd

F32 = mybir.dt.float32
BF16 = mybir.dt.bfloat16
I32 = mybir.dt.int32
AF = mybir.ActivationFunctionType
ALU = mybir.AluOpType
AX = mybir.AxisListType

NTC = 16
NTO = 16
NT = 32
EPS = 1e-6
NEG = -30000.0
import os
KSKIP = set(os.environ.get('KSKIP', '').split(','))
N_EXP = 32


class Op:
    __slots__ = ("e", "fn", "idx", "dma", "dcount", "waits", "need_inc", "inc")

    def __init__(self, e, fn, idx, dma):
        self.e = e
        self.fn = fn
        self.idx = idx
        self.dma = dma
        self.dcount = 0
        self.waits = []
        self.need_inc = False
        self.inc = 0


class Sched:
    ENG = ("pe", "act", "dve", "pool", "sp")

    def __init__(self, nc):
        self.nc = nc
        self.sem = {e: nc.alloc_semaphore("sem_" + e) for e in self.ENG}
        self.cnt = {e: 0 for e in self.ENG}
        self.dsem = {}
        self.batch = set()
        self.nblk = 0
        self.reset()

    def reset(self):
        self.ops = {e: [] for e in self.ENG}
        self.lastw = {}
        self.readers = {}
        self.seen = {}

    def _slot(self, key):
        if key not in self.dsem:
            self.dsem[key] = [self.nc.alloc_semaphore("dsem%d" % len(self.dsem)), 0]
        return self.dsem[key]

    def op(self, e, fn, reads=(), writes=(), dma=None, ndma=1):
        o = Op(e, fn, len(self.ops[e]), dma)
        deps = []
        for k in reads:
            w = self.lastw.get(k)
            if w is not None:
                deps.append((w, True))
        for k in writes:
            w = self.lastw.get(k)
            if w is not None:
                deps.append((w, False))
            rd = self.readers.get(k)
            if rd:
                for r in rd.values():
                    deps.append((r, False))
        for k in reads:
            self.readers.setdefault(k, {})[(e, dma)] = o
        for k in writes:
            self.lastw[k] = o
            self.readers[k] = {}
        waits = []
        for d, raw in deps:
            if d is o:
                continue
            if d.dma is not None:
                key = ("d", d.dma)
                if self.seen.get((e, key), 0) >= d.dcount:
                    continue
                self.seen[(e, key)] = d.dcount
                waits.append(("d", d.dma, d.dcount))
            else:
                if d.e == e:
                    if e in ("pe", "sp"):
                        continue
                    if not raw:
                        continue
                key = ("e", d.e)
                if self.seen.get((e, key), -1) >= d.idx:
                    continue
                self.seen[(e, key)] = d.idx
                d.need_inc = True
                waits.append(("e", d))
        o.waits = waits
        if dma is not None:
            s = self._slot(dma)
            s[1] += 16 * ndma
            o.dcount = s[1]
        self.ops[e].append(o)
        return o

    def flush(self):
        nc = self.nc
        for e in self.ENG:
            for o in self.ops[e]:
                if o.need_inc:
                    self.cnt[e] += 1
                    o.inc = self.cnt[e]
        self.nblk += 1

        def mk(e):
            def body(eng):
                for o in self.ops[e]:
                    for w in o.waits:
                        if w[0] == "d":
                            v = self.dsem[w[1]][1] if w[1] in self.batch else w[2]
                            eng.wait_ge(self.dsem[w[1]][0], v)
                        else:
                            eng.wait_ge(self.sem[w[1].e], w[1].inc)
                    r = o.fn(eng)
                    if o.dma is not None:
                        rl = r if isinstance(r, (list, tuple)) else [r]
                        for ins in rl:
                            ins.then_inc(self.dsem[o.dma][0], 16)
                    elif o.need_inc:
                        r.then_inc(self.sem[e], 1)
                if e == "sp":
                    for key, (sem, c) in self.dsem.items():
                        if c > 0:
                            eng.wait_ge(sem, c)
            return body

        with nc.Block("blk%d" % self.nblk) as blk:
            blk.tensor(mk("pe"))
            blk.scalar(mk("act"))
            blk.vector(mk("dve"))
            blk.gpsimd(mk("pool"))
            blk.sync(mk("sp"))
        self.reset()


def build_program(stage=99, debug=False):
    nc = bass.Bass("TRN2", target_bir_lowering=False)

    def din(name, shape, dt=F32):
        return nc.dram_tensor(name, list(shape), dt, kind="ExternalInput").ap()

    xs = din("xs", [4096, 1024])
    posd = din("pos", [128, NT], I32)
    ctxd = din("ctx", [128, 2])
    ccold = din("ccol", [128, 8])
    n1cold = din("n1col", [128, 8])
    n2cold = din("n2col", [128, 8])
    adabd = din("adab", [1, 6144])
    adawd = din("adaw", [1024, 6144])
    wind = din("w_in", [1024, 3584])
    woutd = din("w_out", [1024, 1024])
    lamvd = din("lamv", [1, 256])
    sublnd = din("subln", [1, 128])
    hlbd = din("hlb", [1, 1024])
    hgnwd = din("hgnw", [1, 128])
    wrtd = din("wrt", [1024, 36])
    brtd = din("brt", [1, 36])
    wgd = din("w_gate", [N_EXP, 1024, 256])
    wud = din("w_up", [N_EXP, 1024, 256])
    wdd = din("w_down", [N_EXP, 256, 1024])
    fnwd = din("fnw", [1, 1024])
    idbd = din("idb", [128, 128], BF16)
    idfd = din("idf", [128, 128])
    triLd = din("triL", [128, 128])
    triUd = din("triU", [128, 128])
    chkd = din("chunkind", [128, 2])
    maskbd = din("maskb", [128, 4, 512], BF16)
    invfd = din("invf", [128, 8])
    outd = nc.dram_tensor("out", [2048, 1024], F32, kind="ExternalOutput").ap()

    dbg_outs = {}

    S = Sched(nc)
    S.batch.update(["c0", "c1"])

    def sb(name, shape, dt=F32):
        return nc.alloc_sbuf_tensor(name, list(shape), dt)

    idb = sb("idb_t", [128, 128], BF16)
    idf = sb("idf_t", [128, 128])
    triL = sb("triL_t", [128, 128])
    triU = sb("triU_t", [128, 128])
    chk = sb("chk_t", [128, 2])
    maskb = sb("maskb_t", [128, 4, 512], BF16)
    ones1 = sb("ones1", [1, 128])
    cosT = sb("cosT", [128, NT, 8])
    sinT = sb("sinT", [128, NT, 8])
    modc = sb("modc", [128, 32])
    A1 = modc[:, 0:8]
    B1 = modc[:, 8:16]
    A2 = modc[:, 16:24]
    B2 = modc[:, 24:32]
    gate1bc = sb("gate1bc", [128, 1024])
    gate2bc = sb("gate2bc", [128, 1024])
    lbbc = sb("lbbc", [128, 512])
    omlbc = sb("omlbc", [128, 512])
    sublnbc = sb("sublnbc", [128, 128])
    hgn4 = sb("hgn4", [128, 4, 128])
    rbbc = sb("rbbc", [128, 36])
    sc = sb("scal", [128, 16])
    ctxb = sc[:, 0:1]
    ctx01 = sc[:, 1:2]
    epsc = sc[:, 2:3]
    neglam = sc[:, 3:4]
    ssqall = sb("ssqall", [128, 320])
    state = sb("state", [128, 4, 128])
    state_bf = [sb("state_bf%d" % i, [128, 4, 128], BF16) for i in range(2)]
    cb = sb("cb", [128, NTO, 32])

    ARENA_F32 = 45056
    arena = sb("arena", [128, ARENA_F32])
    print("sbuf remaining after alloc:", nc.sbuf_bytes_remaining)

    def av(off_bytes, shape, dt=F32):
        esz = 4 if dt in (F32, I32) else 2
        n = int(np.prod(shape[1:]))
        nbytes = n * esz
        assert off_bytes % 4 == 0 and nbytes % 4 == 0
        assert off_bytes + nbytes <= ARENA_F32 * 4, (off_bytes, nbytes)
        v = arena[0:shape[0], off_bytes // 4:(off_bytes + nbytes) // 4]
        if dt != F32:
            v = v.bitcast(dt)
        if len(shape) == 3:
            v = v.rearrange("p (a b) -> p a b", b=shape[2])
        elif len(shape) == 4:
            v = v.rearrange("p (a b c) -> p a b c", b=shape[2], c=shape[3])
        return v

    class Bump:
        def __init__(self, base, limit):
            self.o = base
            self.limit = limit

        def __call__(self, shape, dt=F32):
            esz = 4 if dt in (F32, I32) else 2
            nbytes = int(np.prod(shape[1:])) * esz
            nbytes = (nbytes + 31) // 32 * 32
            v = av(self.o, shape, dt)
            self.o += nbytes
            assert self.o <= self.limit, (self.o, self.limit)
            return v

    R0, R1, R2, R3, REND = 0, 66048, 98816, 131584, ARENA_F32 * 4

    banks = [nc.alloc_psum_tensor("bank%d" % i, [128, 512], F32) for i in range(8)]

    def bk(i):
        return banks[i][:, :]

    def bkbf(i, a, b):
        return banks[i][:, :].bitcast(BF16).rearrange("p (a b) -> p a b", b=b)[:, 0:a, :]

    def DMA(q, out, in_, r, w, slot):
        S.op(q, lambda e: e.dma_start(out=out, in_=in_), reads=r, writes=w, dma=slot)

    def ACT(out, in_, func, r, w, **kw):
        S.op("act", lambda e: e.activation(out=out, in_=in_, func=func, **kw), reads=r, writes=w)

    def TS(eng, out, in0, s1, s2, op0, op1, r, w):
        if op1 is None:
            S.op(eng, lambda e: e.tensor_scalar(out=out, in0=in0, scalar1=s1, scalar2=None, op0=op0), reads=r, writes=w)
        else:
            S.op(eng, lambda e: e.tensor_scalar(out=out, in0=in0, scalar1=s1, scalar2=s2, op0=op0, op1=op1),
                 reads=r, writes=w)

    def TT(eng, out, in0, in1, op, r, w):
        S.op(eng, lambda e: e.tensor_tensor(out=out, in0=in0, in1=in1, op=op), reads=r, writes=w)

    def STT(eng, out, in0, scalar, in1, op0, op1, r, w):
        S.op(eng, lambda e: e.scalar_tensor_tensor(out=out, in0=in0, scalar=scalar, in1=in1, op0=op0, op1=op1),
             reads=r, writes=w)

    def CP(eng, out, in_, r, w):
        if eng == "act":
            S.op("act", lambda e: e.activation(out=out, in_=in_, func=AF.Copy), reads=r, writes=w)
        else:
            S.op(eng, lambda e: e.tensor_copy(out=out, in_=in_), reads=r, writes=w)

    def MM(out, lhsT, rhs, start, stop, r, w, skip=False):
        if skip:
            S.op("pe", lambda e: e.matmul(out=out, lhsT=lhsT, rhs=rhs, start=start, stop=stop, skip_group_check=True),
                 reads=r, writes=w)
        else:
            S.op("pe", lambda e: e.matmul(out=out, lhsT=lhsT, rhs=rhs, start=start, stop=stop), reads=r, writes=w)

    def TR(out, in_, ident, r, w):
        S.op("pe", lambda e: e.transpose(out=out, in_=in_, identity=ident), reads=r, writes=w)

    def RED(eng, out, in_, op, r, w):
        S.op(eng, lambda e: e.tensor_reduce(out=out, in_=in_, axis=AX.X, op=op), reads=r, writes=w)

    def MEMSET(eng, ap, val, w):
        S.op(eng, lambda e: e.memset(ap, val), writes=w)

    def RECIP(out, in_, r, w):
        S.op("dve", lambda e: e.reciprocal(out=out, in_=in_), reads=r, writes=w)

    ssq_next = [0]

    def new_ssq():
        i = ssq_next[0]
        ssq_next[0] += 1
        assert i < 320
        return ssqall[:, i:i + 1], ("ssq", i)

    dbg_list = []

    def DBG(name, ap, shape, key):
        if not debug:
            return
        t = nc.dram_tensor("dbg_" + name, list(shape), ap.dtype, kind="ExternalOutput").ap()
        DMA("sp", t, ap, key, [], "dbg")
        dbg_list.append(name)

    def rstd_from_ssq(ssq, kssq, out, kout, n):
        ACT(out, ssq, AF.Ln, [kssq, "consts"], [kout], scale=1.0 / n, bias=epsc)
        ACT(out, out, AF.Exp, [kout], [kout], scale=-0.5)

    B = Bump(0, REND)
    ccol = B([128, 8])
    cact = B([128, 8])
    n1c = B([128, 8])
    n2c = B([128, 8])
    adab_t = B([1, 6144])
    modrow = B([1, 6144])
    adaw_t = [B([128, 8, 1024]) for _ in range(2)]
    lamt = B([128, 256])
    lamp = B([128, 128])
    hlbt = B([128, 2, 512])
    hlbe = B([128, 2, 512])
    hgt = B([128, 128])
    posi = B([128, NT], I32)
    posf = B([128, NT])
    invf = B([128, 8])
    u_t = B([128, NT, 8])
    uc_t = B([128, NT, 8])
    ki_t = B([128, NT, 8], I32)
    kf_t = B([128, NT, 8])
    sm = B([128, 16])

    for (dst, src, k) in [(idb[:], idbd, "idb"), (idf[:], idfd, "idf"), (triL[:], triLd, "triL"),
                          (triU[:], triUd, "triU"), (chk[:], chkd, "chk"), (maskb[:], maskbd, "maskb"),
                          (sc[:, 0:2], ctxd, "ctx"), (ccol, ccold, "ccol"), (n1c, n1cold, "n1c"),
                          (n2c, n2cold, "n2c"), (adab_t, adabd, "adab"), (posi, posd, "posi"), (invf, invfd, "invf")]:
        DMA("sp", dst, src, [], [k], "c0")
    for (dst, src, k) in [(lamt, lamvd, "lamt"), (hlbt.rearrange("p a b -> p (a b)"), hlbd, "hlbt"),
                          (sublnbc[:], sublnd, "sublnbc"), (hgt, hgnwd, "hgt"), (rbbc[:], brtd, "rbbc")]:
        DMA("sp", dst, src.partition_broadcast(128), [], [k], "c1")
    MEMSET("pool", ones1[:], 1.0, ["ones1"])
    MEMSET("pool", ssqall[:], 0.0, [("ssq", i) for i in range(320)])
    MEMSET("pool", sc[:, 2:3], EPS, ["consts"])
    MEMSET("pool", state[:], 0.0, ["state"])

    ACT(cact, ccol, AF.Silu, ["ccol"], ["cact"])
    for p in range(6):
        sl = p % 2
        DMA("sp", adaw_t[sl], adawd[:, p * 1024:(p + 1) * 1024].rearrange("(k q) n -> q k n", q=128),
            [], [("adaw", sl)], ("adaw", sl))
        for half in range(2):
            bi = (p * 2 + half) % 2
            for kc in range(8):
                MM(banks[bi][0:1, :], cact[:, kc:kc + 1], adaw_t[sl][:, kc, half * 512:(half + 1) * 512],
                   kc == 0, kc == 7, ["cact", ("adaw", sl)], [("bank", bi)])
            c0 = p * 1024 + half * 512
            TT("dve", modrow[0:1, c0:c0 + 512], banks[bi][0:1, :], adab_t[0:1, c0:c0 + 512], ALU.add,
               [("bank", bi), "adab"], [("modrow", p)])
    for idx, p in enumerate([1, 0, 4, 3]):
        for j in range(8):
            c0 = p * 1024 + j * 128
            MM(banks[2][:, idx * 8 + j:idx * 8 + j + 1], modrow[0:1, c0:c0 + 128], ones1[0:1, 0:1], True, True,
               [("modrow", p), "ones1"], [("bank", 2)])
    STT("dve", modc[:, 0:8], banks[2][:, 0:8], 1.0, n1c, ALU.add, ALU.mult, [("bank", 2), "n1c"], ["modc"])
    CP("dve", modc[:, 8:16], banks[2][:, 8:16], [("bank", 2)], ["modc"])
    STT("dve", modc[:, 16:24], banks[2][:, 16:24], 1.0, n2c, ALU.add, ALU.mult, [("bank", 2), "n2c"], ["modc"])
    CP("dve", modc[:, 24:32], banks[2][:, 24:32], [("bank", 2)], ["modc"])
    for gi_, (g, p) in enumerate([(gate1bc, 2), (gate2bc, 5)]):
        for half in range(2):
            bi = 3 + (gi_ * 2 + half) % 2
            c0 = p * 1024 + half * 512
            MM(bk(bi), ones1[0:1, :], modrow[0:1, c0:c0 + 512], True, True, [("modrow", p), "ones1"], [("bank", bi)])
            CP("act", g[:, half * 512:(half + 1) * 512], bk(bi), [("bank", bi)], [("gate", gi_)])
    TT("dve", lamp[:, 0:64], lamt[:, 0:64], lamt[:, 64:128], ALU.mult, ["lamt"], ["lamp"])
    TT("dve", lamp[:, 64:128], lamt[:, 128:192], lamt[:, 192:256], ALU.mult, ["lamt"], ["lamp"])
    RED("dve", sm[:, 0:1], lamp[:, 0:64], ALU.add, ["lamp"], ["sm0"])
    RED("dve", sm[:, 1:2], lamp[:, 64:128], ALU.add, ["lamp"], ["sm0"])
    ACT(sm[:, 2:4], sm[:, 0:2], AF.Exp, ["sm0"], ["sm1"])
    TT("dve", sm[:, 4:5], sm[:, 3:4], sm[:, 2:3], ALU.subtract, ["sm1"], ["sm2"])
    TS("dve", sc[:, 3:4], sm[:, 4:5], -0.2, None, ALU.add, None, ["sm2"], ["neglam"])
    ACT(hlbe, hlbt, AF.Exp, ["hlbt"], ["hlbe"])
    TT("dve", hlbt[:, 0, :], hlbe[:, 0, :], hlbe[:, 1, :], ALU.add, ["hlbe"], ["hlbs"])
    RECIP(hlbt[:, 1, :], hlbt[:, 0, :], ["hlbs"], ["hlbr"])
    TT("dve", lbbc[:], hlbe[:, 0, :], hlbt[:, 1, :], ALU.mult, ["hlbe", "hlbr"], ["lbbc"])
    TS("dve", omlbc[:], lbbc[:], -1.0, 1.0, ALU.mult, ALU.add, ["lbbc"], ["omlbc"])
    TS("dve", sublnbc[:], sublnbc[:], 0.8, None, ALU.mult, None, ["sublnbc"], ["sublnbc"])
    CP("dve", hgn4[:], hgt.unsqueeze(1).to_broadcast([128, 4, 128]), ["hgt"], ["hgn4"])
    CP("dve", posf, posi, ["posi"], ["posf"])
    TT("dve", u_t, posf.unsqueeze(2).to_broadcast([128, NT, 8]), invf.unsqueeze(1).to_broadcast([128, NT, 8]),
       ALU.mult, ["posf", "invf"], ["u"])
    TS("dve", uc_t, u_t, 0.25, None, ALU.add, None, ["u"], ["uc"])
    for (src, ksrc, dst, kdst) in [(u_t, "u", sinT, "sinT"), (uc_t, "uc", cosT, "cosT")]:
        CP("dve", ki_t, src, [ksrc], ["ki"])
        CP("dve", kf_t, ki_t, ["ki"], ["kf"])
        TT("dve", src, src, kf_t, ALU.subtract, [ksrc, "kf"], [ksrc])
        TS("dve", kf_t, src, 0.5, None, ALU.is_gt, None, [ksrc], ["kf"])
        TT("dve", src, src, kf_t, ALU.subtract, [ksrc, "kf"], [ksrc])
        TS("dve", kf_t, src, -0.5, None, ALU.is_lt, None, [ksrc], ["kf"])
        TT("dve", src, src, kf_t, ALU.add, [ksrc, "kf"], [ksrc])
        ACT(dst[:], src, AF.Sin, [ksrc], [kdst], scale=float(2 * np.pi))
    if debug:
        DBG("modc", modc[:], [128, 32], ["modc"])
        DBG("gate1", gate1bc[:], [128, 1024], [("gate", 0)])
        DBG("gate2", gate2bc[:], [128, 1024], [("gate", 1)])
        DBG("cos", cosT[:], [128, NT, 8], ["cosT"])
        DBG("sin", sinT[:], [128, NT, 8], ["sinT"])
        DBG("lb", lbbc[:], [128, 512], ["lbbc"])
        DBG("sc", sc[:], [128, 16], ["neglam", "consts", "ctx"])
    S.flush()
    if stage <= 0:
        return nc, dbg_list

    kT = av(R0, [128, 4, 4096], BF16)
    v_sb = av(R0 + 32768, [128, NT, 4, 130], BF16)
    mixed = av(R1, [128, NTO, 1024], BF16)

    def load_w(cols_list, wt, key):
        for g, c0 in enumerate(cols_list):
            DMA("pool", wt[:, :, g * 512:(g + 1) * 512], wind[:, c0:c0 + 512].rearrange("(k q) n -> q k n", q=128),
                [], [(key, g)], (key, g))

    def front_gen(t, xt, xn, hT):
        sx = t % len(xt)
        sn = t % len(xn)
        sh = t % len(hT)
        kx, kxn, kh = ("xt", sx), ("xn", sn), ("hT", sh)
        DMA("sp", xt[sx], xs[t * 128:(t + 1) * 128, :], [], [kx], ("x", sx))
        yield None
        ssq, kss = new_ssq()
        ACT(xn[sn], xt[sx], AF.Square, [kx, kss], [kxn, kss], accum_out=ssq)
        yield "sub"
        ACT(ssq, ssq, AF.Ln, [kss, "consts"], [kss], scale=1.0 / 1024, bias=epsc)
        yield "sub"
        ACT(ssq, ssq, AF.Exp, [kss], [kss], scale=-0.5)
        yield "sub"
        ACT(xn[sn], xt[sx], AF.Copy, [kx, kss], [kxn], scale=ssq)
        yield None
        psT = bkbf(0, 8, 128)
        for j in range(8):
            TR(psT[:, j, :], xn[sn][:, j * 128:(j + 1) * 128], idb[:], [kxn, "idb"], [("bank", 0)])
        for j in range(8):
            TS("dve", hT[sh][:, j, :], psT[:, j, :], A1[:, j:j + 1], B1[:, j:j + 1], ALU.mult, ALU.add,
               [("bank", 0), "modc"], [kh])
        yield (hT[sh], kh)

    def proj_group(hTt, kh, wt, wkey, g, bank):
        for kc in range(8):
            MM(bk(bank), hTt[:, kc, :], wt[:, kc, g * 512:(g + 1) * 512], kc == 0, kc == 7,
               [kh, (wkey, g)], [("bank", bank)])

    def rope(src_f, ksrc, dst_bf, kdst, ngrp, t, tmp, ktmp):
        sv = src_f.rearrange("p (g d) -> p g d", d=64)
        dv = dst_bf.rearrange("p (g d) -> p g d", d=64)
        cb_ = cosT[:, t, :].unsqueeze(1).to_broadcast([128, ngrp, 8])
        sb_ = sinT[:, t, :].unsqueeze(1).to_broadcast([128, ngrp, 8])
        t1 = sv[:, :, 0:8]
        t2 = sv[:, :, 8:16]
        ta = tmp[:, 0:ngrp, :]
        tb = tmp[:, ngrp:2 * ngrp, :]
        TT("dve", ta, t1, cb_, ALU.mult, [ksrc, "cosT"], [ktmp + "a"])
        TT("dve", tb, t2, sb_, ALU.mult, [ksrc, "sinT"], [ktmp + "b"])
        TT("dve", dv[:, :, 0:8], ta, tb, ALU.subtract, [ktmp + "a", ktmp + "b"], [kdst])
        tc_ = tmp[:, 2 * ngrp:3 * ngrp, :]
        td = tmp[:, 3 * ngrp:4 * ngrp, :]
        TT("dve", tc_, t2, cb_, ALU.mult, [ksrc, "cosT"], [ktmp + "c"])
        TT("dve", td, t1, sb_, ALU.mult, [ksrc, "sinT"], [ktmp + "d"])
        TT("dve", dv[:, :, 8:16], tc_, td, ALU.add, [ktmp + "c", ktmp + "d"], [kdst])

    def SIGMOID(out, in_, r, w):
        ACT(out, in_, AF.Exp, r, w, scale=-1.0)
        TS("dve", out, out, 1.0, None, ALU.add, None, w, w)
        RECIP(out, out, w, w)

    def run_pipeline(make_gen, n):
        active = []
        late = []
        t = 0
        while t < n or active:
            subs = []

            def step(g):
                try:
                    r = next(g)
                except StopIteration:
                    if g in active:
                        active.remove(g)
                    return None
                return r

            def advance_subs():
                for g in list(subs):
                    r = step(g)
                    if r != "sub":
                        subs.remove(g)
                    if r == "late":
                        late.append(g)

            run_late = late[:]
            del late[:]
            for g in list(active):
                if g in run_late:
                    continue
                r = step(g)
                if r == "sub":
                    subs.append(g)
                elif r == "late":
                    late.append(g)
                    advance_subs()
                else:
                    advance_subs()
            if t < n:
                g = make_gen(t)
                active.append(g)
                r = step(g)
                if r == "sub":
                    subs.append(g)
                t += 1
                advance_subs()
            while subs:
                advance_subs()
            for g in run_late:
                while step(g) is not None:
                    pass

    B = Bump(R3, REND)
    xt = [B([128, 1024]) for _ in range(3)]
    xn = [B([128, 1024], BF16) for _ in range(2)]
    hT = [B([128, 8, 128], BF16) for _ in range(2)]
    kf = [B([128, 512]) for _ in range(3)]
    kbf = [B([128, 512], BF16) for _ in range(3)]
    sg_ = [B([128, 512]) for _ in range(2)]
    f_ = [B([128, 512]) for _ in range(2)]
    logf_ = [B([128, 512]) for _ in range(2)]
    B = Bump(R1, R2)
    kk_ = [B([128, 512]) for _ in range(4)]
    er_ = [B([128, 512]) for _ in range(2)]
    kd_ = [B([128, 512], BF16) for _ in range(4)]
    vg_ = [B([128, 512], BF16) for _ in range(8)]
    dec_ = [B([128, 8]) for _ in range(8)]
    rtmp = [B([128, 64, 8]) for _ in range(1)]
    stmp = B([128, 4, 128])
    wA = av(R2, [128, 8, 2048], BF16)
    load_w([512, 1024, 2048, 2560], wA, "wA")
    MEMSET("pool", v_sb[:, :, :, 128:130], 1.0, ["v_ones"])
    TS("dve", v_sb[:, 0:NTC, :, 128:130], v_sb[:, 0:NTC, :, 128:130], ctx01, None, ALU.mult, None, ["v_ones", "ctx"],
       ["v_ones"])

    def tileA(t):
        i_kf, i_sg, i_f, i_lf, i_kk, i_er, i_kd, i_vg, i_dec = (t % 3, t % 2, t % 2, t % 2, t % 4, t % 2, t % 4,
                                                                  t % 8, t % 8)
        fg = front_gen(t, xt, xn, hT)
        r = None
        for r in fg:
            if r is None or r == "sub":
                yield r
        hTt, kh = r
        yield
        for g in range(4):
            proj_group(hTt, kh, wA, "wA", g, 1 + g)
        CP("act", kf[i_kf], bk(1), [("bank", 1)], [("kf", i_kf)])
        CP("pool", kbf[i_kf], kf[i_kf], [("kf", i_kf)], [("kbf", i_kf)])
        ACT(v_sb[:, t, :, 0:128], bk(2).rearrange("p (h d) -> p h d", d=128), AF.Copy, [("bank", 2), "ctx"],
            [("v", t)], scale=ctx01)
        ACT(sg_[i_sg], bk(3), AF.Sigmoid, [("bank", 3)], [("sg", i_sg)])
        CP("act", vg_[i_vg], bk(4), [("bank", 4)], [("vg", i_vg)])
        yield
        rope(kf[i_kf], ("kf", i_kf), kbf[i_kf], ("kbf", i_kf), 8, t, rtmp[0], "rt")
        TT("pool", f_[i_f], sg_[i_sg], omlbc[:], ALU.mult, [("sg", i_sg), "omlbc"], [("f", i_f)])
        TT("pool", f_[i_f], f_[i_f], lbbc[:], ALU.add, [("f", i_f), "lbbc"], [("f", i_f)])
        yield
        ACT(logf_[i_lf], f_[i_f], AF.Ln, [("f", i_f)], [("logf", i_lf)])
        TS("pool", kk_[i_kk], f_[i_f], -1.0, 1.0, ALU.mult, ALU.add, [("f", i_f)], [("kk", i_kk)])
        pk = bkbf(5, 4, 128)
        for h in range(4):
            TR(pk[:, h, :], kbf[i_kf][:, h * 128:(h + 1) * 128], idb[:], [("kbf", i_kf), "idb"], [("bank", 5)])
        CP("act", kT[:, :, t * 128:(t + 1) * 128], pk, [("bank", 5)], [("kT", t)])
        yield
        MM(bk(6), triL[:], logf_[i_lf], True, True, ["triL", ("logf", i_lf)], [("bank", 6)])
        ACT(er_[i_er], bk(6), AF.Exp, [("bank", 6)], [("er", i_er)], scale=-1.0)
        for h in range(4):
            MM(banks[5][:, 256 + 2 * h:256 + 2 * h + 2], logf_[i_lf][:, h * 128:(h + 1) * 128], chk[:], True, True,
               [("logf", i_lf), "chk"], [("bank5b", 0)])
        ACT(dec_[i_dec], banks[5][:, 256:264], AF.Exp, [("bank5b", 0)], [("dec", i_dec)])
        yield
        TT("dve", kd_[i_kd], kk_[i_kk], er_[i_er], ALU.mult, [("kk", i_kk), ("er", i_er)], [("kd", i_kd)])
        yield
        pu = bk(7).rearrange("p (h d) -> p h d", d=128)
        for c in range(2):
            for h in range(4):
                MM(pu[:, h, :], kd_[i_kd][c * 64:(c + 1) * 64, h * 128:(h + 1) * 128],
                   vg_[i_vg][c * 64:(c + 1) * 64, h * 128:(h + 1) * 128], True, True,
                   [("kd", i_kd), ("vg", i_vg)], [("bank", 7)])
            yield "sub"
            TT("dve", stmp[:], state[:], pu, ALU.add, ["state", ("bank", 7)], ["stmp"])
            dbc = dec_[i_dec].rearrange("p (h c) -> p h c", c=2)[:, :, c:c + 1].to_broadcast([128, 4, 128])
            TT("dve", state[:], stmp[:], dbc, ALU.mult, ["stmp", ("dec", i_dec)], ["state"])
            if c == 0:
                yield "sub"

    run_pipeline(tileA, NTC)
    TS("dve", state[:], state[:], ctx01, None, ALU.mult, None, ["state", "ctx"], ["state"])
    CP("act", state_bf[0][:], state[:], ["state"], [("sbf", 0)])
    if debug:
        DBG("kT", kT, [128, 4, 4096], [("kT", t) for t in range(NTC)])
        DBG("v", v_sb, [128, NT, 4, 130], [("v", t) for t in range(NTC)] + ["v_ones"])
        DBG("state", state[:], [128, 4, 128], ["state"])
    S.flush()
    if stage <= 1:
        return nc, dbg_list

    B = Bump(R3, REND)
    qT = B([128, 4, 2048], BF16)
    B1mark = B.o
    xt = [B([128, 1024]) for _ in range(2)]
    xn = [B([128, 1024], BF16) for _ in range(2)]
    hT = [B([128, 8, 128], BF16) for _ in range(2)]
    qkf = [B([128, 1024]) for _ in range(2)]
    qkbf = [B([128, 1024], BF16) for _ in range(2)]
    rtmp = [B([128, 64, 8])]
    wB1 = av(R2, [128, 8, 1536], BF16)
    load_w([0, 512, 1024], wB1, "wB1")

    def tileB1(to):
        t = NTC + to
        s2 = to % 2
        fg = front_gen(t, xt, xn, hT)
        r = None
        for r in fg:
            if r is None or r == "sub":
                yield r
        hTt, kh = r
        yield
        for g in range(3):
            proj_group(hTt, kh, wB1, "wB1", g, 1 + g)
        CP("act", qkf[s2][:, 0:512], bk(1), [("bank", 1)], [("qkf", s2)])
        CP("act", qkf[s2][:, 512:1024], bk(2), [("bank", 2)], [("qkf", s2)])
        CP("pool", qkbf[s2], qkf[s2], [("qkf", s2)], [("qkbf", s2)])
        CP("act", v_sb[:, t, :, 0:128], bk(3).rearrange("p (h d) -> p h d", d=128), [("bank", 3)], [("v", t)])
        yield
        rope(qkf[s2], ("qkf", s2), qkbf[s2], ("qkbf", s2), 16, t, rtmp[0], "rt")
        yield
        pq = bkbf(4, 8, 128)
        for g in range(8):
            TR(pq[:, g, :], qkbf[s2][:, g * 128:(g + 1) * 128], idb[:], [("qkbf", s2), "idb"], [("bank", 4)])
        CP("act", qT[:, :, to * 128:(to + 1) * 128], pq[:, 0:4, :], [("bank", 4)], [("qT", to)])
        CP("act", kT[:, :, t * 128:(t + 1) * 128], pq[:, 4:8, :], [("bank", 4)], [("kT", t)])

    run_pipeline(tileB1, NTO)
    S.flush()

    B = Bump(B1mark, REND)
    pT = [B([128, 512], BF16) for _ in range(5)]
    atmp = [B([128, 128]) for _ in range(2)]
    a2s = [B([128, 4, 128]) for _ in range(2)]
    accsb = [B([128, 3, 396]) for _ in range(2)]
    rl = [B([128, 16]) for _ in range(2)]
    ssq4 = [B([128, 4]) for _ in range(2)]
    pend_fin = [None]
    hcount = [0]
    SB = [0, 1, 5, 6]

    def acc_ap(a):
        bi = 2 + a // 3
        c0 = (a % 3) * 132
        return banks[bi][:, c0:c0 + 129], ("bank", bi)

    for qb in range(4):
        nkt = NTC + 4 * (qb + 1)
        for h in range(4):
            it = [0]

            def qk(kt):
                res = []
                for m in range(2):
                    slot = (it[0]) % 4
                    it[0] += 1
                    sbk = SB[slot]
                    j = kt - (NTC + 4 * qb)
                    diag = j >= 0
                    MM(bk(sbk), kT[m * 64:(m + 1) * 64, h, kt * 128:(kt + 1) * 128],
                       qT[m * 64:(m + 1) * 64, h, qb * 512:(qb + 1) * 512], True, not diag, [], [("bank", sbk)])
                    if diag:
                        MM(bk(sbk), idb[:], maskb[:, j, :], False, True, [], [("bank", sbk)])
                    res.append((slot, sbk))
                return res

            def av_(kt, slots):
                j = kt - (NTC + 4 * qb)
                for m in range(2):
                    slot, sbk = slots[m]
                    ACT(pT[slot], bk(sbk), AF.Exp, [("bank", sbk)], [("pT", slot)], scale=0.125)
                    for qs in range(4):
                        if j > qs:
                            continue
                        last = NTC + 4 * qb + qs
                        a_ = m * 4 + qs
                        acc, kacc = acc_ap(a_)
                        MM(acc, pT[slot][:, qs * 128:(qs + 1) * 128], v_sb[:, kt, h, 0:129],
                           kt == 0 and a_ % 3 == 0, kt == last, [("pT", slot)], [kacc], skip=True)

            prev = qk(0)
            for kt in range(nkt):
                nxt = qk(kt + 1) if kt + 1 < nkt else None
                av_(kt, prev)
                prev = nxt
                if kt == 12 and pend_fin[0] is not None:
                    pend_fin[0]()
                    pend_fin[0] = None
            es = hcount[0] % 2
            hcount[0] += 1
            for bi in range(3):
                CP("dve", accsb[es][:, bi, :], banks[2 + bi][:, 0:396], [("bank", 2 + bi)], [("accsb", es)])

            def sb_acc(a_, es=es):
                return accsb[es][:, a_ // 3, (a_ % 3) * 132:(a_ % 3) * 132 + 129]

            for qs in range(4):
                acc0 = sb_acc(qs)
                acc1 = sb_acc(4 + qs)
                kr_ = ("rl", es)
                RECIP(rl[es][:, 2 * qs:2 * qs + 1], acc0[:, 128:129], [("accsb", es)], [kr_])
                RECIP(rl[es][:, 2 * qs + 1:2 * qs + 2], acc1[:, 128:129], [("accsb", es)], [kr_])
                TT("dve", rl[es][:, 8 + qs:9 + qs], rl[es][:, 2 * qs + 1:2 * qs + 2], neglam, ALU.mult, [kr_], [kr_])
                TS("dve", atmp[0], acc0[:, 0:128], rl[es][:, 2 * qs:2 * qs + 1], None, ALU.mult, None,
                   [("accsb", es), kr_], ["atmp"])
                STT("dve", a2s[es][:, qs, :], acc1[:, 0:128], rl[es][:, 8 + qs:9 + qs], atmp[0], ALU.mult, ALU.add,
                    [("accsb", es), kr_, "atmp"], [("a2s", es)])
                TT("dve", atmp[1], a2s[es][:, qs, :], a2s[es][:, qs, :], ALU.mult, [("a2s", es)], ["atmp1"])
                RED("dve", ssq4[es][:, qs:qs + 1], atmp[1], ALU.add, ["atmp1"], [("ssq4", es)])

            def fin(qb=qb, h=h, es=es):
                rstd_from_ssq(ssq4[es][:], ("ssq4", es), ssq4[es][:], ("ssq4", es), 128)
                for qs in range(4):
                    to = qb * 4 + qs
                    STT("dve", mixed[:, to, h * 128:(h + 1) * 128], a2s[es][:, qs, :], ssq4[es][:, qs:qs + 1],
                        sublnbc[:], ALU.mult, ALU.mult, [("a2s", es), ("ssq4", es), "sublnbc"], [("mixed", to)])

            pend_fin[0] = fin
    if pend_fin[0] is not None:
        pend_fin[0]()
    if debug:
        DBG("mixed_da", mixed, [128, NTO, 1024], [("mixed", i) for i in range(NTO)])
        DBG("kT2", kT, [128, 4, 4096], [])
    S.flush()
    if stage <= 2:
        return nc, dbg_list

    B = Bump(R3, REND)
    xt = [B([128, 1024]) for _ in range(3)]
    xn = [B([128, 1024], BF16) for _ in range(2)]
    hT = [B([128, 8, 128], BF16) for _ in range(3)]
    sgq = [B([128, 512]) for _ in range(2)]
    sg_ = [B([128, 512]) for _ in range(3)]
    sgg = [B([128, 512]) for _ in range(2)]
    f_ = [B([128, 512]) for _ in range(2)]
    logf_ = [B([128, 512]) for _ in range(2)]
    B = Bump(R0, R1)
    kk_ = [B([128, 512]) for _ in range(4)]
    ebx = [B([128, 512]) for _ in range(2)]
    enb = [B([128, 512]) for _ in range(2)]
    osb = [B([128, 512]) for _ in range(2)]
    stmp = B([128, 4, 128])
    qf = [B([128, 512], BF16) for _ in range(6)]
    gw = [B([128, 512], BF16) for _ in range(8)]
    vg_ = [B([128, 512], BF16) for _ in range(8)]
    qbm = [B([128, 512], BF16) for _ in range(2)]
    kbm = [B([128, 512], BF16) for _ in range(4)]
    qbT = [B([128, 4, 128], BF16) for _ in range(2)]
    kbT = [B([128, 4, 128], BF16) for _ in range(2)]
    qbT0 = [B([128, 4, 128], BF16) for _ in range(2)]
    qbT1 = [B([128, 4, 128], BF16) for _ in range(2)]
    scT = [B([128, 4, 128], BF16) for _ in range(2)]
    dec_ = [B([128, 8]) for _ in range(6)]
    hjunk = B([128, 128], BF16)
    ssq4b = [B([128, 4]) for _ in range(2)]
    wB2 = av(R2, [128, 8, 2048], BF16)
    load_w([1536, 2048, 2560, 3072], wB2, "wB2")
    for i in range(2):
        MEMSET("pool", ssq4b[i][:], 0.0, [("ss4", i)])
        MEMSET("pool", qbT0[i][:], 0.0, [("qbT0", i)])
        MEMSET("pool", qbT1[i][:], 0.0, [("qbT1", i)])
    curs = [0]

    def tileB2(to):
        t = NTC + to
        s2 = to % 2
        i_sg, i_qf, i_vg, i_gw, i_kk, i_kb, i_dec = to % 3, to % 6, to % 8, to % 8, to % 4, to % 4, to % 6
        fg = front_gen(t, xt, xn, hT)
        r = None
        for r in fg:
            if r is None or r == "sub":
                yield r
        hTt, kh = r
        yield
        proj_group(hTt, kh, wB2, "wB2", 0, 1)
        proj_group(hTt, kh, wB2, "wB2", 1, 2)
        ACT(sgq[s2], bk(1), AF.Sigmoid, [("bank", 1)], [("sgq", s2)])
        ACT(sg_[i_sg], bk(2), AF.Sigmoid, [("bank", 2)], [("sg", i_sg)])
        TT("dve", qf[i_qf], bk(1), sgq[s2], ALU.mult, [("bank", 1), ("sgq", s2)], [("qf", i_qf)])
        yield
        proj_group(hTt, kh, wB2, "wB2", 2, 1)
        proj_group(hTt, kh, wB2, "wB2", 3, 2)
        CP("act", vg_[i_vg], bk(1), [("bank", 1)], [("vg", i_vg)])
        ACT(sgg[s2], bk(2), AF.Sigmoid, [("bank", 2)], [("sgg", s2)])
        TT("dve", sgg[s2], bk(2), sgg[s2], ALU.mult, [("bank", 2), ("sgg", s2)], [("sgg", s2)])
        TT("pool", gw[i_gw], sgg[s2], hgn4[:].rearrange("p h d -> p (h d)"), ALU.mult, [("sgg", s2), "hgn4"],
           [("gw", i_gw)])
        TT("pool", f_[s2], sg_[i_sg], omlbc[:], ALU.mult, [("sg", i_sg), "omlbc"], [("f", s2)])
        TT("pool", f_[s2], f_[s2], lbbc[:], ALU.add, [("f", s2), "lbbc"], [("f", s2)])
        yield
        ACT(logf_[s2], f_[s2], AF.Ln, [("f", s2)], [("logf", s2)])
        TS("pool", kk_[i_kk], f_[s2], -1.0, 1.0, ALU.mult, ALU.add, [("f", s2)], [("kk", i_kk)])
        yield
        MM(bk(5), triL[:], logf_[s2], True, True, ["triL", ("logf", s2)], [("bank", 5)])
        for h in range(4):
            MM(banks[4][:, 2 * h:2 * h + 2], logf_[s2][:, h * 128:(h + 1) * 128], chk[:], True, True,
               [("logf", s2), "chk"], [("bank", 4)])
        ACT(ebx[s2], bk(5), AF.Exp, [("bank", 5)], [("ebx", s2)])
        ACT(enb[s2], bk(5), AF.Exp, [("bank", 5)], [("enb", s2)], scale=-1.0)
        ACT(dec_[i_dec], banks[4][:, 0:8], AF.Exp, [("bank", 4)], [("dec", i_dec)])
        yield
        TT("dve", qbm[s2], qf[i_qf], ebx[s2], ALU.mult, [("qf", i_qf), ("ebx", s2)], [("qbm", s2)])
        TT("pool", kbm[i_kb], kk_[i_kk], enb[s2], ALU.mult, [("kk", i_kk), ("enb", s2)], [("kbm", i_kb)])
        yield
        pq = bkbf(3, 8, 128)
        for h in range(4):
            TR(pq[:, h, :], qbm[s2][:, h * 128:(h + 1) * 128], idb[:], [("qbm", s2), "idb"], [("bank", 3)])
        for h in range(4):
            TR(pq[:, 4 + h, :], kbm[i_kb][:, h * 128:(h + 1) * 128], idb[:], [("kbm", i_kb), "idb"], [("bank", 3)])
        CP("act", qbT[s2][:], pq[:, 0:4, :], [("bank", 3)], [("qbT", s2)])
        CP("act", kbT[s2][:], pq[:, 4:8, :], [("bank", 3)], [("kbT", s2)])
        yield
        CP("pool", qbT0[s2][:, :, 0:64], qbT[s2][:, :, 0:64], [("qbT", s2)], [("qbT0", s2)])
        CP("pool", qbT1[s2][:, :, 64:128], qbT[s2][:, :, 64:128], [("qbT", s2)], [("qbT1", s2)])
        psc = bk(6).rearrange("p (h d) -> p h d", d=128)
        for h in range(4):
            MM(psc[:, h, :], kbT[s2][:, h, :], qbT[s2][:, h, :], True, True, [("kbT", s2), ("qbT", s2)], [("bank", 6)])
        yield "sub"
        TT("dve", scT[s2][:], psc, triL[:].unsqueeze(1).to_broadcast([128, 4, 128]), ALU.mult,
           [("bank", 6), "triL"], [("scT", s2)])
        yield "sub"
        po = psc
        pu = bk(7).rearrange("p (h d) -> p h d", d=128)
        for h in range(4):
            MM(po[:, h, :], scT[s2][:, h, :], vg_[i_vg][:, h * 128:(h + 1) * 128], h == 0, False,
               [("scT", s2), ("vg", i_vg)], [("bank", 6)], skip=True)
        for c in range(2):
            cur = curs[0]
            for h in range(4):
                MM(pu[:, h, :], kbm[i_kb][c * 64:(c + 1) * 64, h * 128:(h + 1) * 128],
                   vg_[i_vg][c * 64:(c + 1) * 64, h * 128:(h + 1) * 128], True, True,
                   [("kbm", i_kb), ("vg", i_vg)], [("bank", 7)])
            qc = qbT0[s2] if c == 0 else qbT1[s2]
            kqc = ("qbT0", s2) if c == 0 else ("qbT1", s2)
            for h in range(4):
                MM(po[:, h, :], qc[:, h, :], state_bf[cur][:, h, :], False, c == 1, [kqc, ("sbf", cur)],
                   [("bank", 6)], skip=True)
            yield "sub"
            TT("dve", stmp[:], state[:], pu, ALU.add, ["state", ("bank", 7)], ["stmp"])
            dbc = dec_[i_dec].rearrange("p (h c) -> p h c", c=2)[:, :, c:c + 1].to_broadcast([128, 4, 128])
            TT("dve", state[:], stmp[:], dbc, ALU.mult, ["stmp", ("dec", i_dec)], ["state"])
            curs[0] = 1 - cur
            yield "sub"
            CP("act", state_bf[1 - cur][:], state[:], ["state"], [("sbf", 1 - cur)])
            if c == 0:
                yield "sub"
        CP("act", osb[s2], bk(6), [("bank", 6)], [("osb", s2)])
        yield "late"
        ss4 = ssq4b[s2]
        for h in range(4):
            ACT(hjunk, osb[s2][:, h * 128:(h + 1) * 128], AF.Square, [("osb", s2), ("ss4", s2)], ["hjunk", ("ss4", s2)],
                accum_out=ss4[:, h:h + 1])
        rstd_from_ssq(ss4[:], ("ss4", s2), ss4[:], ("ss4", s2), 128)
        yield "sub"
        for h in range(4):
            STT("dve", mixed[:, to, 512 + h * 128:512 + (h + 1) * 128], osb[s2][:, h * 128:(h + 1) * 128],
                ss4[:, h:h + 1], gw[i_gw][:, h * 128:(h + 1) * 128], ALU.mult, ALU.mult,
                [("osb", s2), ("ss4", s2), ("gw", i_gw)], [("mixedh", to)])
        MEMSET("pool", ss4[:], 0.0, [("ss4", s2)])

    run_pipeline(tileB2, NTO)
    if debug:
        DBG("mixed_all", mixed, [128, NTO, 1024], [("mixedh", i) for i in range(NTO)])
    S.flush()
    if stage <= 3:
        return nc, dbg_list

    y_acc = av(R0, [128, NTO, 1024])
    B = Bump(R2, REND)
    wo_bf = B([128, 8, 1024], BF16)
    wo_st = [B([128, 4, 1024]) for _ in range(1)]
    xt = [B([128, 1024]) for _ in range(4)]
    mT = [B([128, 8, 128], BF16) for _ in range(3)]
    for half in range(2):
        DMA("sp", wo_st[0], woutd[half * 512:(half + 1) * 512, :].rearrange("(k q) n -> q k n", q=128),
            [], ["wo_st"], "wo_st")
        for j in range(4):
            eng = "dve" if j % 2 == 0 else "pool"
            TT(eng, wo_bf[:, half * 4 + j, :], wo_st[0][:, j, :], gate1bc[:], ALU.mult, ["wo_st"],
               [("wo", half * 4 + j)])
    def tileC1(to):
        sl = to % 3
        sx = to % 4
        t = NTC + to
        DMA("sp", xt[sx], xs[t * 128:(t + 1) * 128, :], [], [("xt", sx)], ("x", sx))
        yield
        pm = bkbf(to % 2, 8, 128)
        for j in range(8):
            TR(pm[:, j, :], mixed[:, to, j * 128:(j + 1) * 128], idb[:], ["idb"], [("bank", to % 2)])
        CP("act", mT[sl][:], pm, [("bank", to % 2)], [("mT", sl)])
        yield
        yield
        for half in range(2):
            bi = 2 + (to % 2) * 2 + half
            for j in range(8):
                MM(bk(bi), mT[sl][:, j, :], wo_bf[:, j, half * 512:(half + 1) * 512], j == 0, j == 7,
                   [("mT", sl), ("wo", j)], [("bank", bi)])
            TT("dve", y_acc[:, to, half * 512:(half + 1) * 512], bk(bi), xt[sx][:, half * 512:(half + 1) * 512],
               ALU.add, [("bank", bi), ("xt", sx)], [("y", to)])

    run_pipeline(tileC1, NTO)
    if debug:
        DBG("x1", y_acc, [128, NTO, 1024], [("y", i) for i in range(NTO)])
    S.flush()
    if stage <= 4:
        return nc, dbg_list

    h2T = av(R2, [128, 8, 2048], BF16)
    B = Bump(R1, R2)
    xn2 = [B([128, 1024]) for _ in range(2)]
    h2f = [B([128, 8, 128]) for _ in range(3)]
    wrt = B([128, 8, 36])
    junk2 = B([128, 1024], BF16)
    lgG = B([128, NTO, 4])
    lgE = B([128, NTO, 32])
    pen = B([128, NTO, 4])
    em = B([128, NTO, 32])
    oh = B([128, NTO, 32])
    r16 = [B([128, NTO]) for _ in range(8)]
    DMA("sp", wrt, wrtd.rearrange("(k q) n -> q k n", q=128), [], ["wrt"], "wrt")
    def tileC2(to):
        sl = to % 2
        sh = to % 3
        ssq, kss = new_ssq()
        ACT(junk2, y_acc[:, to, :], AF.Square, [kss], ["junk2", kss], accum_out=ssq)
        rstd_from_ssq(ssq, kss, ssq, kss, 1024)
        ACT(xn2[sl], y_acc[:, to, :], AF.Copy, [kss], [("xn2", sl)], scale=ssq)
        yield
        for j in range(8):
            bi = (to % 2) * 2 + j // 4
            TR(banks[bi][:, (j % 4) * 128:(j % 4 + 1) * 128], xn2[sl][:, j * 128:(j + 1) * 128], idf[:],
               [("xn2", sl), "idf"], [("bank", bi)])
        for j in range(8):
            bi = (to % 2) * 2 + j // 4
            TS("dve", h2f[sh][:, j, :], banks[bi][:, (j % 4) * 128:(j % 4 + 1) * 128], A2[:, j:j + 1], B2[:, j:j + 1],
               ALU.mult, ALU.add, [("bank", bi)], [("h2f", sh)])
        CP("pool", h2T[:, :, to * 128:(to + 1) * 128], h2f[sh][:], [("h2f", sh)], [("h2T", to // 4)])
        yield
        yield
        lb_ = 4 + to % 2
        for j in range(8):
            MM(banks[lb_][:, 0:36], h2f[sh][:, j, :], wrt[:, j, :], j == 0, j == 7, [("h2f", sh), "wrt"],
               [("bank", lb_)])
        TT("dve", lgG[:, to, :], banks[lb_][:, 0:4], rbbc[:, 0:4], ALU.add, [("bank", lb_)], ["lgG"])
        TT("dve", lgE[:, to, :], banks[lb_][:, 4:36], rbbc[:, 4:36], ALU.add, [("bank", lb_)], ["lgE"])

    run_pipeline(tileC2, NTO)
    gmax, gsum, gwt, m1, m2, dlt, coef, tmp16 = r16

    def bc(ap16, n):
        return ap16.unsqueeze(2).to_broadcast([128, NTO, n])

    RED("dve", gmax, lgG, ALU.max, ["lgG"], ["gmax"])
    TT("dve", pen, lgG, bc(gmax, 4), ALU.is_equal, ["lgG", "gmax"], ["pen"])
    TT("dve", lgG, lgG, bc(gmax, 4), ALU.subtract, ["lgG", "gmax", "pen"], ["lgG"])
    ACT(lgG, lgG, AF.Exp, ["lgG"], ["lgG"])
    RED("dve", gsum, lgG, ALU.add, ["lgG"], ["gsum"])
    RECIP(gwt, gsum, ["gsum"], ["gwt"])
    TS("dve", pen, pen, 1e30, -1e30, ALU.mult, ALU.add, ["pen"], ["pen"])
    TT("dve", em.rearrange("p t (g e) -> p (t g) e", e=8), lgE.rearrange("p t (g e) -> p (t g) e", e=8),
       pen.rearrange("p t g -> p (t g)").unsqueeze(2).to_broadcast([128, NTO * 4, 8]), ALU.add, ["lgE", "pen"], ["em"])
    RED("dve", m1, em, ALU.max, ["em"], ["m1"])
    TT("dve", oh, em, bc(m1, 32), ALU.is_equal, ["em", "m1"], ["oh"])
    STT("dve", oh, oh, -1e30, em, ALU.mult, ALU.add, ["oh", "em"], ["oh"])
    RED("dve", m2, oh, ALU.max, ["oh"], ["m2"])
    TT("dve", oh, em, bc(m2, 32), ALU.is_ge, ["em", "m2", "oh"], ["oh"])
    TT("dve", em, em, bc(m1, 32), ALU.subtract, ["em", "m1", "oh"], ["em"])
    ACT(em, em, AF.Exp, ["em"], ["em"])
    TT("dve", dlt, m2, m1, ALU.subtract, ["m1", "m2"], ["dlt"])
    ACT(dlt, dlt, AF.Exp, ["dlt"], ["dlt"])
    TS("dve", dlt, dlt, 1.0, None, ALU.add, None, ["dlt"], ["dlt"])
    RECIP(coef, dlt, ["dlt"], ["coef"])
    TT("dve", coef, coef, gwt, ALU.mult, ["coef", "gwt"], ["coef"])
    TT("dve", em, em, bc(coef, 32), ALU.mult, ["em", "coef"], ["em"])
    TT("dve", cb[:], em, oh, ALU.mult, ["em", "oh"], ["cb"])
    if debug:
        DBG("cb", cb[:], [128, NTO, 32], ["cb"])
    S.flush()
    if stage <= 5:
        return nc, dbg_list

    B = Bump(R1, R2)
    wgu = [B([128, 8, 512], BF16) for _ in range(2)]
    actT = [B([128, 2, 512], BF16) for _ in range(2)]
    sgl = [B([128, 512], BF16) for _ in range(2)]
    fnwbc = B([128, 1024])
    B = Bump(R3, REND)
    wd_st = [B([128, 2, 1024]) for _ in range(2)]
    wd_bf = [B([128, 2, 1024], BF16) for _ in range(2)]
    ojunk = B([128, 1024], BF16)
    DMA("sp", fnwbc, fnwd.partition_broadcast(128), [], ["fnw"], "fnw")

    def load_expert(e):
        sl = e % 2
        DMA("pool", wgu[sl][:, :, 0:256], wgd[e].rearrange("(k q) f -> q k f", q=128), [], [("wg", sl)], ("wg", sl))
        DMA("pool", wgu[sl][:, :, 256:512], wud[e].rearrange("(k q) f -> q k f", q=128), [], [("wu", sl)], ("wu", sl))
        DMA("sp", wd_st[sl], wdd[e].rearrange("(c q) d -> q c d", q=128), [], [("wds", sl)], ("wds", sl))
        for c in range(2):
            TT("pool", wd_bf[sl][:, c, :], wd_st[sl][:, c, :], gate2bc[:], ALU.mult, [("wds", sl)], [("wd", sl)])

    def gate_up(e, tb):
        sl = e % 2
        asl = tb % 2
        for fc in [0, 2, 1, 3]:
            for kc in range(8):
                MM(bk(fc), wgu[sl][:, kc, fc * 128:(fc + 1) * 128], h2T[:, kc, tb * 512:(tb + 1) * 512], kc == 0, kc == 7,
                   [("wg", sl), ("wu", sl), ("h2T", tb)], [("bank", fc)])
        for fc in range(2):
            ACT(sgl[fc], bk(fc), AF.Silu, [("bank", fc)], [("sgl", fc)])
            TT("dve", actT[asl][:, fc, :], sgl[fc], bk(2 + fc), ALU.mult, [("sgl", fc), ("bank", 2 + fc)],
               [("actT", asl)])

    def down(e, tb):
        sl = e % 2
        asl = tb % 2
        for ti in range(4):
            to = tb * 4 + ti
            for half in range(2):
                bi = 4 + (ti * 2 + half) % 4
                for fc in range(2):
                    MM(bk(bi), actT[asl][:, fc, ti * 128:(ti + 1) * 128], wd_bf[sl][:, fc, half * 512:(half + 1) * 512],
                       fc == 0, fc == 1, [("actT", asl), ("wd", sl)], [("bank", bi)])
                STT("dve", y_acc[:, to, half * 512:(half + 1) * 512], bk(bi), cb[:, to, e:e + 1],
                    y_acc[:, to, half * 512:(half + 1) * 512], ALU.mult, ALU.add, [("bank", bi), "cb"], [("y", to)])

    load_expert(0)
    pending = None
    n_exp = N_EXP
    for e in range(n_exp):
        for tb in range(4):
            gate_up(e, tb)
            if pending is not None:
                down(*pending)
            pending = (e, tb)
            if tb == 0 and e + 1 < n_exp:
                load_expert(e + 1)
    down(*pending)
    for to in range(NTO):
        ssq, kss = new_ssq()
        ACT(ojunk, y_acc[:, to, :], AF.Square, [("y", to), kss], ["ojunk", kss], accum_out=ssq)
        rstd_from_ssq(ssq, kss, ssq, kss, 1024)
        STT("dve", y_acc[:, to, :], y_acc[:, to, :], ssq, fnwbc, ALU.mult, ALU.mult, [("y", to), kss, "fnw"],
            [("y", to)])
        DMA("sp", outd[to * 128:(to + 1) * 128, :], y_acc[:, to, :], [("y", to)], [], ("out", to % 4))
    S.flush()
    return nc, dbg_list


def _consts():
    s = np.arange(128)
    same = (s[:, None] // 64) == (s[None, :] // 64)
    triL = (same & (s[:, None] <= s[None, :])).astype(np.float32)
    triU = (same & (s[:, None] > s[None, :])).astype(np.float32)
    chunkind = ((s[:, None] // 64) == np.arange(2)[None, :]).astype(np.float32)
    k = np.arange(128)[:, None, None]
    j = np.arange(4)[None, :, None]
    q = np.arange(512)[None, None, :]
    maskb = np.where(j * 128 + k <= q, 0.0, NEG).astype(np.float32).astype(ml_dtypes.bfloat16)
    half = 8
    inv_freq = (500000.0 ** (-np.arange(half, dtype=np.float32) / half)).astype(np.float32)
    invf = np.broadcast_to((inv_freq / np.float32(2 * np.pi)).astype(np.float32)[None, :], (128, 8)).copy()
    return dict(idb=np.eye(128, dtype=np.float32).astype(ml_dtypes.bfloat16), idf=np.eye(128, dtype=np.float32),
                triL=triL, triU=triU, chunkind=chunkind, maskb=maskb, invf=invf)


def col8(v):
    return np.ascontiguousarray(np.asarray(v, dtype=np.float32).reshape(8, 128).T)


def make_in_maps(inp):
    x = np.asarray(inp["x"], dtype=np.float32)
    pos = np.asarray(inp["positions"]).astype(np.int32)
    cst = _consts()
    shared = dict(
        n1col=col8(inp["norm1_w"][0]), n2col=col8(inp["norm2_w"][0]),
        adab=np.ascontiguousarray(np.asarray(inp["ada_b"], np.float32)[0][None, :]),
        adaw=np.ascontiguousarray(np.asarray(inp["ada_w"], np.float32)[0]),
        w_in=np.ascontiguousarray(np.asarray(inp["w_in"], np.float32)[0]),
        w_out=np.ascontiguousarray(np.asarray(inp["w_out"], np.float32)[0]),
        lamv=np.concatenate([np.asarray(inp[k], np.float32)[0] for k in
                             ("da_lambda_q1", "da_lambda_k1", "da_lambda_q2", "da_lambda_k2")])[None, :].copy(),
        subln=np.ascontiguousarray(np.asarray(inp["da_subln_w"], np.float32)[0][None, :]),
        hlb=np.ascontiguousarray(np.asarray(inp["hg_lower_bound"], np.float32).reshape(1, 1024)),
        hgnw=np.ascontiguousarray(np.asarray(inp["hg_norm_w"], np.float32)[0][None, :]),
        wrt=np.ascontiguousarray(np.concatenate([np.asarray(inp["moe_w_group"], np.float32)[0],
                                                 np.asarray(inp["moe_w_router"], np.float32)[0]], axis=1)),
        brt=np.ascontiguousarray(np.concatenate([np.asarray(inp["moe_b_group"], np.float32)[0],
                                                 np.asarray(inp["moe_b_router"], np.float32)[0]])[None, :]),
        w_gate=np.ascontiguousarray(np.asarray(inp["moe_w_gate"], np.float32)[0]),
        w_up=np.ascontiguousarray(np.asarray(inp["moe_w_up"], np.float32)[0]),
        w_down=np.ascontiguousarray(np.asarray(inp["moe_w_down"], np.float32)[0]),
        fnw=np.ascontiguousarray(np.asarray(inp["final_norm_w"], np.float32)[None, :]),
        **cst,
    )
    maps = []
    for i in range(8):
        b, half = i // 2, i % 2
        if half == 0:
            xs_ = np.concatenate([x[b, 0:2048], x[b, 0:2048]], axis=0)
            ps_ = np.concatenate([pos[b, 0:2048], pos[b, 0:2048]])
            ctx = np.tile(np.array([[NEG, 0.0]], np.float32), (128, 1))
        else:
            xs_ = x[b]
            ps_ = pos[b]
            ctx = np.tile(np.array([[0.0, 1.0]], np.float32), (128, 1))
        m = dict(shared)
        m["xs"] = np.ascontiguousarray(xs_)
        m["pos"] = np.ascontiguousarray(ps_.reshape(NT, 128).T)
        m["ctx"] = ctx
        m["ccol"] = col8(np.asarray(inp["c"], np.float32)[b])
        maps.append(m)
    return maps


_CACHE = {}


def kernel(**inputs):
    maps = make_in_maps(inputs)
    if "nc" not in _CACHE:
        _CACHE["nc"] = build_program()[0]
    nc = _CACHE["nc"]
    res = run_bass_kernel_spmd(nc, maps, core_ids=list(range(8)))
    out = np.empty((4, 4096, 1024), np.float32)
    for i in range(8):
        b, half = i // 2, i % 2
        out[b, half * 2048:(half + 1) * 2048] = res.results[i]["out"]
    return out
```

```python
import numpy as np
import ml_dtypes
import concourse.bass as bass
import concourse.mybir as mybir
from concourse.bass_utils import run_bass_kernel_spmd

F32 = mybir.dt.float32
BF16 = mybir.dt.bfloat16
I32 = mybir.dt.int32
AF = mybir.ActivationFunctionType
ALU = mybir.AluOpType
AX = mybir.AxisListType

NTC = 16
NTO = 16
NT = 32
EPS = 1e-6
NEG = -30000.0
import os
KSKIP = set(os.environ.get('KSKIP', '').split(','))
N_EXP = 32


class Op:
    __slots__ = ("e", "fn", "idx", "dma", "dcount", "waits", "need_inc", "inc")

    def __init__(self, e, fn, idx, dma):
        self.e = e
        self.fn = fn
        self.idx = idx
        self.dma = dma
        self.dcount = 0
        self.waits = []
        self.need_inc = False
        self.inc = 0


class Sched:
    ENG = ("pe", "act", "dve", "pool", "sp")

    def __init__(self, nc):
        self.nc = nc
        self.sem = {e: nc.alloc_semaphore("sem_" + e) for e in self.ENG}
        self.cnt = {e: 0 for e in self.ENG}
        self.dsem = {}
        self.batch = set()
        self.nblk = 0
        self.reset()

    def reset(self):
        self.ops = {e: [] for e in self.ENG}
        self.lastw = {}
        self.readers = {}
        self.seen = {}

    def _slot(self, key):
        if key not in self.dsem:
            self.dsem[key] = [self.nc.alloc_semaphore("dsem%d" % len(self.dsem)), 0]
        return self.dsem[key]

    def op(self, e, fn, reads=(), writes=(), dma=None, ndma=1):
        o = Op(e, fn, len(self.ops[e]), dma)
        deps = []
        for k in reads:
            w = self.lastw.get(k)
            if w is not None:
                deps.append((w, True))
        for k in writes:
            w = self.lastw.get(k)
            if w is not None:
                deps.append((w, False))
            rd = self.readers.get(k)
            if rd:
                for r in rd.values():
                    deps.append((r, False))
        for k in reads:
            self.readers.setdefault(k, {})[(e, dma)] = o
        for k in writes:
            self.lastw[k] = o
            self.readers[k] = {}
        waits = []
        for d, raw in deps:
            if d is o:
                continue
            if d.dma is not None:
                key = ("d", d.dma)
                if self.seen.get((e, key), 0) >= d.dcount:
                    continue
                self.seen[(e, key)] = d.dcount
                waits.append(("d", d.dma, d.dcount))
            else:
                if d.e == e:
                    if e in ("pe", "sp"):
                        continue
                    if not raw:
                        continue
                key = ("e", d.e)
                if self.seen.get((e, key), -1) >= d.idx:
                    continue
                self.seen[(e, key)] = d.idx
                d.need_inc = True
                waits.append(("e", d))
        o.waits = waits
        if dma is not None:
            s = self._slot(dma)
            s[1] += 16 * ndma
            o.dcount = s[1]
        self.ops[e].append(o)
        return o

    def flush(self):
        nc = self.nc
        for e in self.ENG:
            for o in self.ops[e]:
                if o.need_inc:
                    self.cnt[e] += 1
                    o.inc = self.cnt[e]
        self.nblk += 1

        def mk(e):
            def body(eng):
                for o in self.ops[e]:
                    for w in o.waits:
                        if w[0] == "d":
                            v = self.dsem[w[1]][1] if w[1] in self.batch else w[2]
                            eng.wait_ge(self.dsem[w[1]][0], v)
                        else:
                            eng.wait_ge(self.sem[w[1].e], w[1].inc)
                    r = o.fn(eng)
                    if o.dma is not None:
                        rl = r if isinstance(r, (list, tuple)) else [r]
                        for ins in rl:
                            ins.then_inc(self.dsem[o.dma][0], 16)
                    elif o.need_inc:
                        r.then_inc(self.sem[e], 1)
                if e == "sp":
                    for key, (sem, c) in self.dsem.items():
                        if c > 0:
                            eng.wait_ge(sem, c)
            return body

        with nc.Block("blk%d" % self.nblk) as blk:
            blk.tensor(mk("pe"))
            blk.scalar(mk("act"))
            blk.vector(mk("dve"))
            blk.gpsimd(mk("pool"))
            blk.sync(mk("sp"))
        self.reset()


def build_program(stage=99, debug=False):
    nc = bass.Bass("TRN2", target_bir_lowering=False)

    def din(name, shape, dt=F32):
        return nc.dram_tensor(name, list(shape), dt, kind="ExternalInput").ap()

    xs = din("xs", [4096, 1024])
    posd = din("pos", [128, NT], I32)
    ctxd = din("ctx", [128, 2])
    ccold = din("ccol", [128, 8])
    n1cold = din("n1col", [128, 8])
    n2cold = din("n2col", [128, 8])
    adabd = din("adab", [1, 6144])
    adawd = din("adaw", [1024, 6144])
    wind = din("w_in", [1024, 3584])
    woutd = din("w_out", [1024, 1024])
    lamvd = din("lamv", [1, 256])
    sublnd = din("subln", [1, 128])
    hlbd = din("hlb", [1, 1024])
    hgnwd = din("hgnw", [1, 128])
    wrtd = din("wrt", [1024, 36])
    brtd = din("brt", [1, 36])
    wgd = din("w_gate", [N_EXP, 1024, 256])
    wud = din("w_up", [N_EXP, 1024, 256])
    wdd = din("w_down", [N_EXP, 256, 1024])
    fnwd = din("fnw", [1, 1024])
    idbd = din("idb", [128, 128], BF16)
    idfd = din("idf", [128, 128])
    triLd = din("triL", [128, 128])
    triUd = din("triU", [128, 128])
    chkd = din("chunkind", [128, 2])
    maskbd = din("maskb", [128, 4, 512], BF16)
    invfd = din("invf", [128, 8])
    outd = nc.dram_tensor("out", [2048, 1024], F32, kind="ExternalOutput").ap()

    dbg_outs = {}

    S = Sched(nc)
    S.batch.update(["c0", "c1"])

    def sb(name, shape, dt=F32):
        return nc.alloc_sbuf_tensor(name, list(shape), dt)

    idb = sb("idb_t", [128, 128], BF16)
    idf = sb("idf_t", [128, 128])
    triL = sb("triL_t", [128, 128])
    triU = sb("triU_t", [128, 128])
    chk = sb("chk_t", [128, 2])
    maskb = sb("maskb_t", [128, 4, 512], BF16)
    ones1 = sb("ones1", [1, 128])
    cosT = sb("cosT", [128, NT, 8])
    sinT = sb("sinT", [128, NT, 8])
    modc = sb("modc", [128, 32])
    A1 = modc[:, 0:8]
    B1 = modc[:, 8:16]
    A2 = modc[:, 16:24]
    B2 = modc[:, 24:32]
    gate1bc = sb("gate1bc", [128, 1024])
    gate2bc = sb("gate2bc", [128, 1024])
    lbbc = sb("lbbc", [128, 512])
    omlbc = sb("omlbc", [128, 512])
    sublnbc = sb("sublnbc", [128, 128])
    hgn4 = sb("hgn4", [128, 4, 128])
    rbbc = sb("rbbc", [128, 36])
    sc = sb("scal", [128, 16])
    ctxb = sc[:, 0:1]
    ctx01 = sc[:, 1:2]
    epsc = sc[:, 2:3]
    neglam = sc[:, 3:4]
    ssqall = sb("ssqall", [128, 320])
    state = sb("state", [128, 4, 128])
    state_bf = [sb("state_bf%d" % i, [128, 4, 128], BF16) for i in range(2)]
    cb = sb("cb", [128, NTO, 32])

    ARENA_F32 = 45056
    arena = sb("arena", [128, ARENA_F32])
    print("sbuf remaining after alloc:", nc.sbuf_bytes_remaining)

    def av(off_bytes, shape, dt=F32):
        esz = 4 if dt in (F32, I32) else 2
        n = int(np.prod(shape[1:]))
        nbytes = n * esz
        assert off_bytes % 4 == 0 and nbytes % 4 == 0
        assert off_bytes + nbytes <= ARENA_F32 * 4, (off_bytes, nbytes)
        v = arena[0:shape[0], off_bytes // 4:(off_bytes + nbytes) // 4]
        if dt != F32:
            v = v.bitcast(dt)
        if len(shape) == 3:
            v = v.rearrange("p (a b) -> p a b", b=shape[2])
        elif len(shape) == 4:
            v = v.rearrange("p (a b c) -> p a b c", b=shape[2], c=shape[3])
        return v

    class Bump:
        def __init__(self, base, limit):
            self.o = base
            self.limit = limit

        def __call__(self, shape, dt=F32):
            esz = 4 if dt in (F32, I32) else 2
            nbytes = int(np.prod(shape[1:])) * esz
            nbytes = (nbytes + 31) // 32 * 32
            v = av(self.o, shape, dt)
            self.o += nbytes
            assert self.o <= self.limit, (self.o, self.limit)
            return v

    R0, R1, R2, R3, REND = 0, 66048, 98816, 131584, ARENA_F32 * 4

    banks = [nc.alloc_psum_tensor("bank%d" % i, [128, 512], F32) for i in range(8)]

    def bk(i):
        return banks[i][:, :]

    def bkbf(i, a, b):
        return banks[i][:, :].bitcast(BF16).rearrange("p (a b) -> p a b", b=b)[:, 0:a, :]

    def DMA(q, out, in_, r, w, slot):
        S.op(q, lambda e: e.dma_start(out=out, in_=in_), reads=r, writes=w, dma=slot)

    def ACT(out, in_, func, r, w, **kw):
        S.op("act", lambda e: e.activation(out=out, in_=in_, func=func, **kw), reads=r, writes=w)

    def TS(eng, out, in0, s1, s2, op0, op1, r, w):
        if op1 is None:
            S.op(eng, lambda e: e.tensor_scalar(out=out, in0=in0, scalar1=s1, scalar2=None, op0=op0), reads=r, writes=w)
        else:
            S.op(eng, lambda e: e.tensor_scalar(out=out, in0=in0, scalar1=s1, scalar2=s2, op0=op0, op1=op1),
                 reads=r, writes=w)

    def TT(eng, out, in0, in1, op, r, w):
        S.op(eng, lambda e: e.tensor_tensor(out=out, in0=in0, in1=in1, op=op), reads=r, writes=w)

    def STT(eng, out, in0, scalar, in1, op0, op1, r, w):
        S.op(eng, lambda e: e.scalar_tensor_tensor(out=out, in0=in0, scalar=scalar, in1=in1, op0=op0, op1=op1),
             reads=r, writes=w)

    def CP(eng, out, in_, r, w):
        if eng == "act":
            S.op("act", lambda e: e.activation(out=out, in_=in_, func=AF.Copy), reads=r, writes=w)
        else:
            S.op(eng, lambda e: e.tensor_copy(out=out, in_=in_), reads=r, writes=w)

    def MM(out, lhsT, rhs, start, stop, r, w, skip=False):
        if skip:
            S.op("pe", lambda e: e.matmul(out=out, lhsT=lhsT, rhs=rhs, start=start, stop=stop, skip_group_check=True),
                 reads=r, writes=w)
        else:
            S.op("pe", lambda e: e.matmul(out=out, lhsT=lhsT, rhs=rhs, start=start, stop=stop), reads=r, writes=w)

    def TR(out, in_, ident, r, w):
        S.op("pe", lambda e: e.transpose(out=out, in_=in_, identity=ident), reads=r, writes=w)

    def RED(eng, out, in_, op, r, w):
        S.op(eng, lambda e: e.tensor_reduce(out=out, in_=in_, axis=AX.X, op=op), reads=r, writes=w)

    def MEMSET(eng, ap, val, w):
        S.op(eng, lambda e: e.memset(ap, val), writes=w)

    def RECIP(out, in_, r, w):
        S.op("dve", lambda e: e.reciprocal(out=out, in_=in_), reads=r, writes=w)

    ssq_next = [0]

    def new_ssq():
        i = ssq_next[0]
        ssq_next[0] += 1
        assert i < 320
        return ssqall[:, i:i + 1], ("ssq", i)

    dbg_list = []

    def DBG(name, ap, shape, key):
        if not debug:
            return
        t = nc.dram_tensor("dbg_" + name, list(shape), ap.dtype, kind="ExternalOutput").ap()
        DMA("sp", t, ap, key, [], "dbg")
        dbg_list.append(name)

    def rstd_from_ssq(ssq, kssq, out, kout, n):
        ACT(out, ssq, AF.Ln, [kssq, "consts"], [kout], scale=1.0 / n, bias=epsc)
        ACT(out, out, AF.Exp, [kout], [kout], scale=-0.5)

    B = Bump(0, REND)
    ccol = B([128, 8])
    cact = B([128, 8])
    n1c = B([128, 8])
    n2c = B([128, 8])
    adab_t = B([1, 6144])
    modrow = B([1, 6144])
    adaw_t = [B([128, 8, 1024]) for _ in range(2)]
    lamt = B([128, 256])
    lamp = B([128, 128])
    hlbt = B([128, 2, 512])
    hlbe = B([128, 2, 512])
    hgt = B([128, 128])
    posi = B([128, NT], I32)
    posf = B([128, NT])
    invf = B([128, 8])
    u_t = B([128, NT, 8])
    uc_t = B([128, NT, 8])
    ki_t = B([128, NT, 8], I32)
    kf_t = B([128, NT, 8])
    sm = B([128, 16])

    for (dst, src, k) in [(idb[:], idbd, "idb"), (idf[:], idfd, "idf"), (triL[:], triLd, "triL"),
                          (triU[:], triUd, "triU"), (chk[:], chkd, "chk"), (maskb[:], maskbd, "maskb"),
                          (sc[:, 0:2], ctxd, "ctx"), (ccol, ccold, "ccol"), (n1c, n1cold, "n1c"),
                          (n2c, n2cold, "n2c"), (adab_t, adabd, "adab"), (posi, posd, "posi"), (invf, invfd, "invf")]:
        DMA("sp", dst, src, [], [k], "c0")
    for (dst, src, k) in [(lamt, lamvd, "lamt"), (hlbt.rearrange("p a b -> p (a b)"), hlbd, "hlbt"),
                          (sublnbc[:], sublnd, "sublnbc"), (hgt, hgnwd, "hgt"), (rbbc[:], brtd, "rbbc")]:
        DMA("sp", dst, src.partition_broadcast(128), [], [k], "c1")
    MEMSET("pool", ones1[:], 1.0, ["ones1"])
    MEMSET("pool", ssqall[:], 0.0, [("ssq", i) for i in range(320)])
    MEMSET("pool", sc[:, 2:3], EPS, ["consts"])
    MEMSET("pool", state[:], 0.0, ["state"])

    ACT(cact, ccol, AF.Silu, ["ccol"], ["cact"])
    for p in range(6):
        sl = p % 2
        DMA("sp", adaw_t[sl], adawd[:, p * 1024:(p + 1) * 1024].rearrange("(k q) n -> q k n", q=128),
            [], [("adaw", sl)], ("adaw", sl))
        for half in range(2):
            bi = (p * 2 + half) % 2
            for kc in range(8):
                MM(banks[bi][0:1, :], cact[:, kc:kc + 1], adaw_t[sl][:, kc, half * 512:(half + 1) * 512],
                   kc == 0, kc == 7, ["cact", ("adaw", sl)], [("bank", bi)])
            c0 = p * 1024 + half * 512
            TT("dve", modrow[0:1, c0:c0 + 512], banks[bi][0:1, :], adab_t[0:1, c0:c0 + 512], ALU.add,
               [("bank", bi), "adab"], [("modrow", p)])
    for idx, p in enumerate([1, 0, 4, 3]):
        for j in range(8):
            c0 = p * 1024 + j * 128
            MM(banks[2][:, idx * 8 + j:idx * 8 + j + 1], modrow[0:1, c0:c0 + 128], ones1[0:1, 0:1], True, True,
               [("modrow", p), "ones1"], [("bank", 2)])
    STT("dve", modc[:, 0:8], banks[2][:, 0:8], 1.0, n1c, ALU.add, ALU.mult, [("bank", 2), "n1c"], ["modc"])
    CP("dve", modc[:, 8:16], banks[2][:, 8:16], [("bank", 2)], ["modc"])
    STT("dve", modc[:, 16:24], banks[2][:, 16:24], 1.0, n2c, ALU.add, ALU.mult, [("bank", 2), "n2c"], ["modc"])
    CP("dve", modc[:, 24:32], banks[2][:, 24:32], [("bank", 2)], ["modc"])
    for gi_, (g, p) in enumerate([(gate1bc, 2), (gate2bc, 5)]):
        for half in range(2):
            bi = 3 + (gi_ * 2 + half) % 2
            c0 = p * 1024 + half * 512
            MM(bk(bi), ones1[0:1, :], modrow[0:1, c0:c0 + 512], True, True, [("modrow", p), "ones1"], [("bank", bi)])
            CP("act", g[:, half * 512:(half + 1) * 512], bk(bi), [("bank", bi)], [("gate", gi_)])
    TT("dve", lamp[:, 0:64], lamt[:, 0:64], lamt[:, 64:128], ALU.mult, ["lamt"], ["lamp"])
    TT("dve", lamp[:, 64:128], lamt[:, 128:192], lamt[:, 192:256], ALU.mult, ["lamt"], ["lamp"])
    RED("dve", sm[:, 0:1], lamp[:, 0:64], ALU.add, ["lamp"], ["sm0"])
    RED("dve", sm[:, 1:2], lamp[:, 64:128], ALU.add, ["lamp"], ["sm0"])
    ACT(sm[:, 2:4], sm[:, 0:2], AF.Exp, ["sm0"], ["sm1"])
    TT("dve", sm[:, 4:5], sm[:, 3:4], sm[:, 2:3], ALU.subtract, ["sm1"], ["sm2"])
    TS("dve", sc[:, 3:4], sm[:, 4:5], -0.2, None, ALU.add, None, ["sm2"], ["neglam"])
    ACT(hlbe, hlbt, AF.Exp, ["hlbt"], ["hlbe"])
    TT("dve", hlbt[:, 0, :], hlbe[:, 0, :], hlbe[:, 1, :], ALU.add, ["hlbe"], ["hlbs"])
    RECIP(hlbt[:, 1, :], hlbt[:, 0, :], ["hlbs"], ["hlbr"])
    TT("dve", lbbc[:], hlbe[:, 0, :], hlbt[:, 1, :], ALU.mult, ["hlbe", "hlbr"], ["lbbc"])
    TS("dve", omlbc[:], lbbc[:], -1.0, 1.0, ALU.mult, ALU.add, ["lbbc"], ["omlbc"])
    TS("dve", sublnbc[:], sublnbc[:], 0.8, None, ALU.mult, None, ["sublnbc"], ["sublnbc"])
    CP("dve", hgn4[:], hgt.unsqueeze(1).to_broadcast([128, 4, 128]), ["hgt"], ["hgn4"])
    CP("dve", posf, posi, ["posi"], ["posf"])
    TT("dve", u_t, posf.unsqueeze(2).to_broadcast([128, NT, 8]), invf.unsqueeze(1).to_broadcast([128, NT, 8]),
       ALU.mult, ["posf", "invf"], ["u"])
    TS("dve", uc_t, u_t, 0.25, None, ALU.add, None, ["u"], ["uc"])
    for (src, ksrc, dst, kdst) in [(u_t, "u", sinT, "sinT"), (uc_t, "uc", cosT, "cosT")]:
        CP("dve", ki_t, src, [ksrc], ["ki"])
        CP("dve", kf_t, ki_t, ["ki"], ["kf"])
        TT("dve", src, src, kf_t, ALU.subtract, [ksrc, "kf"], [ksrc])
        TS("dve", kf_t, src, 0.5, None, ALU.is_gt, None, [ksrc], ["kf"])
        TT("dve", src, src, kf_t, ALU.subtract, [ksrc, "kf"], [ksrc])
        TS("dve", kf_t, src, -0.5, None, ALU.is_lt, None, [ksrc], ["kf"])
        TT("dve", src, src, kf_t, ALU.add, [ksrc, "kf"], [ksrc])
        ACT(dst[:], src, AF.Sin, [ksrc], [kdst], scale=float(2 * np.pi))
    if debug:
        DBG("modc", modc[:], [128, 32], ["modc"])
        DBG("gate1", gate1bc[:], [128, 1024], [("gate", 0)])
        DBG("gate2", gate2bc[:], [128, 1024], [("gate", 1)])
        DBG("cos", cosT[:], [128, NT, 8], ["cosT"])
        DBG("sin", sinT[:], [128, NT, 8], ["sinT"])
        DBG("lb", lbbc[:], [128, 512], ["lbbc"])
        DBG("sc", sc[:], [128, 16], ["neglam", "consts", "ctx"])
    S.flush()
    if stage <= 0:
        return nc, dbg_list

    kT = av(R0, [128, 4, 4096], BF16)
    v_sb = av(R0 + 32768, [128, NT, 4, 130], BF16)
    mixed = av(R1, [128, NTO, 1024], BF16)

    def load_w(cols_list, wt, key):
        for g, c0 in enumerate(cols_list):
            DMA("pool", wt[:, :, g * 512:(g + 1) * 512], wind[:, c0:c0 + 512].rearrange("(k q) n -> q k n", q=128),
                [], [(key, g)], (key, g))

    def front_gen(t, xt, xn, hT):
        sx = t % len(xt)
        sn = t % len(xn)
        sh = t % len(hT)
        kx, kxn, kh = ("xt", sx), ("xn", sn), ("hT", sh)
        DMA("sp", xt[sx], xs[t * 128:(t + 1) * 128, :], [], [kx], ("x", sx))
        yield None
        ssq, kss = new_ssq()
        ACT(xn[sn], xt[sx], AF.Square, [kx, kss], [kxn, kss], accum_out=ssq)
        yield "sub"
        ACT(ssq, ssq, AF.Ln, [kss, "consts"], [kss], scale=1.0 / 1024, bias=epsc)
        yield "sub"
        ACT(ssq, ssq, AF.Exp, [kss], [kss], scale=-0.5)
        yield "sub"
        ACT(xn[sn], xt[sx], AF.Copy, [kx, kss], [kxn], scale=ssq)
        yield None
        psT = bkbf(0, 8, 128)
        for j in range(8):
            TR(psT[:, j, :], xn[sn][:, j * 128:(j + 1) * 128], idb[:], [kxn, "idb"], [("bank", 0)])
        for j in range(8):
            TS("dve", hT[sh][:, j, :], psT[:, j, :], A1[:, j:j + 1], B1[:, j:j + 1], ALU.mult, ALU.add,
               [("bank", 0), "modc"], [kh])
        yield (hT[sh], kh)

    def proj_group(hTt, kh, wt, wkey, g, bank):
        for kc in range(8):
            MM(bk(bank), hTt[:, kc, :], wt[:, kc, g * 512:(g + 1) * 512], kc == 0, kc == 7,
               [kh, (wkey, g)], [("bank", bank)])

    def rope(src_f, ksrc, dst_bf, kdst, ngrp, t, tmp, ktmp):
        sv = src_f.rearrange("p (g d) -> p g d", d=64)
        dv = dst_bf.rearrange("p (g d) -> p g d", d=64)
        cb_ = cosT[:, t, :].unsqueeze(1).to_broadcast([128, ngrp, 8])
        sb_ = sinT[:, t, :].unsqueeze(1).to_broadcast([128, ngrp, 8])
        t1 = sv[:, :, 0:8]
        t2 = sv[:, :, 8:16]
        ta = tmp[:, 0:ngrp, :]
        tb = tmp[:, ngrp:2 * ngrp, :]
        TT("dve", ta, t1, cb_, ALU.mult, [ksrc, "cosT"], [ktmp + "a"])
        TT("dve", tb, t2, sb_, ALU.mult, [ksrc, "sinT"], [ktmp + "b"])
        TT("dve", dv[:, :, 0:8], ta, tb, ALU.subtract, [ktmp + "a", ktmp + "b"], [kdst])
        tc_ = tmp[:, 2 * ngrp:3 * ngrp, :]
        td = tmp[:, 3 * ngrp:4 * ngrp, :]
        TT("dve", tc_, t2, cb_, ALU.mult, [ksrc, "cosT"], [ktmp + "c"])
        TT("dve", td, t1, sb_, ALU.mult, [ksrc, "sinT"], [ktmp + "d"])
        TT("dve", dv[:, :, 8:16], tc_, td, ALU.add, [ktmp + "c", ktmp + "d"], [kdst])

    def SIGMOID(out, in_, r, w):
        ACT(out, in_, AF.Exp, r, w, scale=-1.0)
        TS("dve", out, out, 1.0, None, ALU.add, None, w, w)
        RECIP(out, out, w, w)

    def run_pipeline(make_gen, n, prio=None):
        active = []
        late = []
        nst = {}
        t = 0
        while t < n or active:
            subs = []

            def step(g):
                try:
                    r = next(g)
                except StopIteration:
                    if g in active:
                        active.remove(g)
                    return None
                return r

            def advance_subs():
                for g in list(subs):
                    r = step(g)
                    if r != "sub":
                        subs.remove(g)
                    if r == "late":
                        late.append(g)

            run_late = late[:]
            del late[:]
            order = list(active)
            if prio is not None:
                order.sort(key=lambda g: prio.index(nst[id(g)]) if nst[id(g)] in prio else len(prio))
            for g in order:
                if g in run_late:
                    continue
                nst[id(g)] += 1
                r = step(g)
                if r == "sub":
                    subs.append(g)
                elif r == "late":
                    late.append(g)
                    advance_subs()
                else:
                    advance_subs()
            if t < n:
                g = make_gen(t)
                active.append(g)
                nst[id(g)] = 1
                r = step(g)
                if r == "sub":
                    subs.append(g)
                t += 1
                advance_subs()
            while subs:
                advance_subs()
            for g in run_late:
                while step(g) is not None:
                    pass

    B = Bump(R3, REND)
    xt = [B([128, 1024]) for _ in range(3)]
    xn = [B([128, 1024], BF16) for _ in range(2)]
    hT = [B([128, 8, 128], BF16) for _ in range(2)]
    kf = [B([128, 512]) for _ in range(3)]
    kbf = [B([128, 512], BF16) for _ in range(3)]
    sg_ = [B([128, 512]) for _ in range(2)]
    f_ = [B([128, 512]) for _ in range(2)]
    logf_ = [B([128, 512]) for _ in range(2)]
    B = Bump(R1, R2)
    kk_ = [B([128, 512]) for _ in range(4)]
    er_ = [B([128, 512]) for _ in range(2)]
    kd_ = [B([128, 512], BF16) for _ in range(4)]
    vg_ = [B([128, 512], BF16) for _ in range(8)]
    dec_ = [B([128, 8]) for _ in range(8)]
    rtmp = [B([128, 64, 8]) for _ in range(1)]
    stmp = B([128, 4, 128])
    wA = av(R2, [128, 8, 2048], BF16)
    load_w([512, 1024, 2048, 2560], wA, "wA")
    MEMSET("pool", v_sb[:, :, :, 128:130], 1.0, ["v_ones"])
    TS("dve", v_sb[:, 0:NTC, :, 128:130], v_sb[:, 0:NTC, :, 128:130], ctx01, None, ALU.mult, None, ["v_ones", "ctx"],
       ["v_ones"])

    def tileA(t):
        i_kf, i_sg, i_f, i_lf, i_kk, i_er, i_kd, i_vg, i_dec = (t % 3, t % 2, t % 2, t % 2, t % 4, t % 2, t % 4,
                                                                  t % 8, t % 8)
        fg = front_gen(t, xt, xn, hT)
        r = None
        for r in fg:
            if r is None or r == "sub":
                yield r
        hTt, kh = r
        yield
        for g in range(4):
            proj_group(hTt, kh, wA, "wA", g, 1 + g)
        CP("act", kf[i_kf], bk(1), [("bank", 1)], [("kf", i_kf)])
        CP("pool", kbf[i_kf], kf[i_kf], [("kf", i_kf)], [("kbf", i_kf)])
        ACT(v_sb[:, t, :, 0:128], bk(2).rearrange("p (h d) -> p h d", d=128), AF.Copy, [("bank", 2), "ctx"],
            [("v", t)], scale=ctx01)
        ACT(sg_[i_sg], bk(3), AF.Sigmoid, [("bank", 3)], [("sg", i_sg)])
        CP("act", vg_[i_vg], bk(4), [("bank", 4)], [("vg", i_vg)])
        yield
        rope(kf[i_kf], ("kf", i_kf), kbf[i_kf], ("kbf", i_kf), 8, t, rtmp[0], "rt")
        TT("pool", f_[i_f], sg_[i_sg], omlbc[:], ALU.mult, [("sg", i_sg), "omlbc"], [("f", i_f)])
        TT("pool", f_[i_f], f_[i_f], lbbc[:], ALU.add, [("f", i_f), "lbbc"], [("f", i_f)])
        yield
        ACT(logf_[i_lf], f_[i_f], AF.Ln, [("f", i_f)], [("logf", i_lf)])
        TS("pool", kk_[i_kk], f_[i_f], -1.0, 1.0, ALU.mult, ALU.add, [("f", i_f)], [("kk", i_kk)])
        pk = bkbf(5, 4, 128)
        for h in range(4):
            TR(pk[:, h, :], kbf[i_kf][:, h * 128:(h + 1) * 128], idb[:], [("kbf", i_kf), "idb"], [("bank", 5)])
        CP("act", kT[:, :, t * 128:(t + 1) * 128], pk, [("bank", 5)], [("kT", t)])
        yield
        MM(bk(6), triL[:], logf_[i_lf], True, True, ["triL", ("logf", i_lf)], [("bank", 6)])
        ACT(er_[i_er], bk(6), AF.Exp, [("bank", 6)], [("er", i_er)], scale=-1.0)
        for h in range(4):
            MM(banks[5][:, 256 + 2 * h:256 + 2 * h + 2], logf_[i_lf][:, h * 128:(h + 1) * 128], chk[:], True, True,
               [("logf", i_lf), "chk"], [("bank5b", 0)])
        ACT(dec_[i_dec], banks[5][:, 256:264], AF.Exp, [("bank5b", 0)], [("dec", i_dec)])
        yield
        TT("dve", kd_[i_kd], kk_[i_kk], er_[i_er], ALU.mult, [("kk", i_kk), ("er", i_er)], [("kd", i_kd)])
        yield
        pu = bk(7).rearrange("p (h d) -> p h d", d=128)
        for c in range(2):
            for h in range(4):
                MM(pu[:, h, :], kd_[i_kd][c * 64:(c + 1) * 64, h * 128:(h + 1) * 128],
                   vg_[i_vg][c * 64:(c + 1) * 64, h * 128:(h + 1) * 128], True, True,
                   [("kd", i_kd), ("vg", i_vg)], [("bank", 7)])
            yield "sub"
            TT("dve", stmp[:], state[:], pu, ALU.add, ["state", ("bank", 7)], ["stmp"])
            dbc = dec_[i_dec].rearrange("p (h c) -> p h c", c=2)[:, :, c:c + 1].to_broadcast([128, 4, 128])
            TT("dve", state[:], stmp[:], dbc, ALU.mult, ["stmp", ("dec", i_dec)], ["state"])
            if c == 0:
                yield "sub"

    run_pipeline(tileA, NTC)
    TS("dve", state[:], state[:], ctx01, None, ALU.mult, None, ["state", "ctx"], ["state"])
    CP("act", state_bf[0][:], state[:], ["state"], [("sbf", 0)])
    if debug:
        DBG("kT", kT, [128, 4, 4096], [("kT", t) for t in range(NTC)])
        DBG("v", v_sb, [128, NT, 4, 130], [("v", t) for t in range(NTC)] + ["v_ones"])
        DBG("state", state[:], [128, 4, 128], ["state"])
    S.flush()
    if stage <= 1:
        return nc, dbg_list

    B = Bump(R3, REND)
    qT = B([128, 4, 2048], BF16)
    B1mark = B.o
    xt = [B([128, 1024]) for _ in range(2)]
    xn = [B([128, 1024], BF16) for _ in range(2)]
    hT = [B([128, 8, 128], BF16) for _ in range(2)]
    qkf = [B([128, 1024]) for _ in range(2)]
    qkbf = [B([128, 1024], BF16) for _ in range(2)]
    rtmp = [B([128, 64, 8])]
    wB1 = av(R2, [128, 8, 1536], BF16)
    load_w([0, 512, 1024], wB1, "wB1")

    def tileB1(to):
        t = NTC + to
        s2 = to % 2
        fg = front_gen(t, xt, xn, hT)
        r = None
        for r in fg:
            if r is None or r == "sub":
                yield r
        hTt, kh = r
        yield
        for g in range(3):
            proj_group(hTt, kh, wB1, "wB1", g, 1 + g)
        CP("act", qkf[s2][:, 0:512], bk(1), [("bank", 1)], [("qkf", s2)])
        CP("act", qkf[s2][:, 512:1024], bk(2), [("bank", 2)], [("qkf", s2)])
        CP("pool", qkbf[s2], qkf[s2], [("qkf", s2)], [("qkbf", s2)])
        CP("act", v_sb[:, t, :, 0:128], bk(3).rearrange("p (h d) -> p h d", d=128), [("bank", 3)], [("v", t)])
        yield
        rope(qkf[s2], ("qkf", s2), qkbf[s2], ("qkbf", s2), 16, t, rtmp[0], "rt")
        yield
        pq = bkbf(4, 8, 128)
        for g in range(8):
            TR(pq[:, g, :], qkbf[s2][:, g * 128:(g + 1) * 128], idb[:], [("qkbf", s2), "idb"], [("bank", 4)])
        CP("act", qT[:, :, to * 128:(to + 1) * 128], pq[:, 0:4, :], [("bank", 4)], [("qT", to)])
        CP("act", kT[:, :, t * 128:(t + 1) * 128], pq[:, 4:8, :], [("bank", 4)], [("kT", t)])

    run_pipeline(tileB1, NTO)
    S.flush()

    B = Bump(B1mark, REND)
    pT = [B([128, 512], BF16) for _ in range(5)]
    atmp = [B([128, 128]) for _ in range(2)]
    a2s = [B([128, 4, 128]) for _ in range(2)]
    accsb = [B([128, 3, 396]) for _ in range(2)]
    rl = [B([128, 16]) for _ in range(2)]
    ssq4 = [B([128, 4]) for _ in range(2)]
    pend_fin = [None]
    hcount = [0]
    SB = [0, 1, 5, 6]

    def acc_ap(a):
        bi = 2 + a // 3
        c0 = (a % 3) * 132
        return banks[bi][:, c0:c0 + 129], ("bank", bi)

    for qb in range(4):
        nkt = NTC + 4 * (qb + 1)
        for h in range(4):
            it = [0]

            def qk(kt):
                res = []
                for m in range(2):
                    slot = (it[0]) % 4
                    it[0] += 1
                    sbk = SB[slot]
                    j = kt - (NTC + 4 * qb)
                    diag = j >= 0
                    MM(bk(sbk), kT[m * 64:(m + 1) * 64, h, kt * 128:(kt + 1) * 128],
                       qT[m * 64:(m + 1) * 64, h, qb * 512:(qb + 1) * 512], True, not diag, [], [("bank", sbk)])
                    if diag:
                        MM(bk(sbk), idb[:], maskb[:, j, :], False, True, [], [("bank", sbk)])
                    res.append((slot, sbk))
                return res

            def av_(kt, slots):
                j = kt - (NTC + 4 * qb)
                for m in range(2):
                    slot, sbk = slots[m]
                    ACT(pT[slot], bk(sbk), AF.Exp, [("bank", sbk)], [("pT", slot)], scale=0.125)
                    for qs in range(4):
                        if j > qs:
                            continue
                        last = NTC + 4 * qb + qs
                        a_ = m * 4 + qs
                        acc, kacc = acc_ap(a_)
                        MM(acc, pT[slot][:, qs * 128:(qs + 1) * 128], v_sb[:, kt, h, 0:129],
                           kt == 0 and a_ % 3 == 0, kt == last, [("pT", slot)], [kacc], skip=True)

            prev = qk(0)
            for kt in range(nkt):
                nxt = qk(kt + 1) if kt + 1 < nkt else None
                av_(kt, prev)
                prev = nxt
                if kt == 12 and pend_fin[0] is not None:
                    pend_fin[0]()
                    pend_fin[0] = None
            es = hcount[0] % 2
            hcount[0] += 1
            for bi in range(3):
                CP("dve", accsb[es][:, bi, :], banks[2 + bi][:, 0:396], [("bank", 2 + bi)], [("accsb", es)])

            def sb_acc(a_, es=es):
                return accsb[es][:, a_ // 3, (a_ % 3) * 132:(a_ % 3) * 132 + 129]

            for qs in range(4):
                acc0 = sb_acc(qs)
                acc1 = sb_acc(4 + qs)
                kr_ = ("rl", es)
                RECIP(rl[es][:, 2 * qs:2 * qs + 1], acc0[:, 128:129], [("accsb", es)], [kr_])
                RECIP(rl[es][:, 2 * qs + 1:2 * qs + 2], acc1[:, 128:129], [("accsb", es)], [kr_])
                TT("dve", rl[es][:, 8 + qs:9 + qs], rl[es][:, 2 * qs + 1:2 * qs + 2], neglam, ALU.mult, [kr_], [kr_])
                TS("dve", atmp[0], acc0[:, 0:128], rl[es][:, 2 * qs:2 * qs + 1], None, ALU.mult, None,
                   [("accsb", es), kr_], ["atmp"])
                STT("dve", a2s[es][:, qs, :], acc1[:, 0:128], rl[es][:, 8 + qs:9 + qs], atmp[0], ALU.mult, ALU.add,
                    [("accsb", es), kr_, "atmp"], [("a2s", es)])
                TT("dve", atmp[1], a2s[es][:, qs, :], a2s[es][:, qs, :], ALU.mult, [("a2s", es)], ["atmp1"])
                RED("dve", ssq4[es][:, qs:qs + 1], atmp[1], ALU.add, ["atmp1"], [("ssq4", es)])

            def fin(qb=qb, h=h, es=es):
                rstd_from_ssq(ssq4[es][:], ("ssq4", es), ssq4[es][:], ("ssq4", es), 128)
                for qs in range(4):
                    to = qb * 4 + qs
                    STT("dve", mixed[:, to, h * 128:(h + 1) * 128], a2s[es][:, qs, :], ssq4[es][:, qs:qs + 1],
                        sublnbc[:], ALU.mult, ALU.mult, [("a2s", es), ("ssq4", es), "sublnbc"], [("mixed", to)])

            pend_fin[0] = fin
    if pend_fin[0] is not None:
        pend_fin[0]()
    if debug:
        DBG("mixed_da", mixed, [128, NTO, 1024], [("mixed", i) for i in range(NTO)])
        DBG("kT2", kT, [128, 4, 4096], [])
    S.flush()
    if stage <= 2:
        return nc, dbg_list

    B = Bump(R3, REND)
    xt = [B([128, 1024]) for _ in range(3)]
    xn = [B([128, 1024], BF16) for _ in range(2)]
    hT = [B([128, 8, 128], BF16) for _ in range(3)]
    sgq = [B([128, 512]) for _ in range(2)]
    sg_ = [B([128, 512]) for _ in range(3)]
    sgg = [B([128, 512]) for _ in range(2)]
    f_ = [B([128, 512]) for _ in range(2)]
    logf_ = [B([128, 512]) for _ in range(2)]
    B = Bump(R0, R1)
    kk_ = [B([128, 512]) for _ in range(4)]
    ebx = [B([128, 512]) for _ in range(2)]
    enb = [B([128, 512]) for _ in range(2)]
    osb = [B([128, 512]) for _ in range(2)]
    stmp = B([128, 4, 128])
    qf = [B([128, 512], BF16) for _ in range(6)]
    gw = [B([128, 512], BF16) for _ in range(8)]
    vg_ = [B([128, 512], BF16) for _ in range(8)]
    qbm = [B([128, 512], BF16) for _ in range(2)]
    kbm = [B([128, 512], BF16) for _ in range(4)]
    qbT = [B([128, 4, 128], BF16) for _ in range(2)]
    kbT = [B([128, 4, 128], BF16) for _ in range(2)]
    qbT0 = [B([128, 4, 128], BF16) for _ in range(2)]
    qbT1 = [B([128, 4, 128], BF16) for _ in range(2)]
    scT = [B([128, 4, 128], BF16) for _ in range(2)]
    dec_ = [B([128, 8]) for _ in range(6)]
    hjunk = B([128, 128], BF16)
    ssq4b = [B([128, 4]) for _ in range(2)]
    wB2 = av(R2, [128, 8, 2048], BF16)
    load_w([1536, 2048, 2560, 3072], wB2, "wB2")
    for i in range(2):
        MEMSET("pool", ssq4b[i][:], 0.0, [("ss4", i)])
        MEMSET("pool", qbT0[i][:], 0.0, [("qbT0", i)])
        MEMSET("pool", qbT1[i][:], 0.0, [("qbT1", i)])
    curs = [0]

    def tileB2(to):
        t = NTC + to
        s2 = to % 2
        i_sg, i_qf, i_vg, i_gw, i_kk, i_kb, i_dec = to % 3, to % 6, to % 8, to % 8, to % 4, to % 4, to % 6
        fg = front_gen(t, xt, xn, hT)
        r = None
        for r in fg:
            if r is None or r == "sub":
                yield r
        hTt, kh = r
        yield
        proj_group(hTt, kh, wB2, "wB2", 0, 1)
        proj_group(hTt, kh, wB2, "wB2", 1, 2)
        ACT(sgq[s2], bk(1), AF.Sigmoid, [("bank", 1)], [("sgq", s2)])
        ACT(sg_[i_sg], bk(2), AF.Sigmoid, [("bank", 2)], [("sg", i_sg)])
        TT("dve", qf[i_qf], bk(1), sgq[s2], ALU.mult, [("bank", 1), ("sgq", s2)], [("qf", i_qf)])
        yield
        proj_group(hTt, kh, wB2, "wB2", 2, 1)
        proj_group(hTt, kh, wB2, "wB2", 3, 2)
        CP("act", vg_[i_vg], bk(1), [("bank", 1)], [("vg", i_vg)])
        ACT(sgg[s2], bk(2), AF.Sigmoid, [("bank", 2)], [("sgg", s2)])
        TT("dve", sgg[s2], bk(2), sgg[s2], ALU.mult, [("bank", 2), ("sgg", s2)], [("sgg", s2)])
        TT("pool", gw[i_gw], sgg[s2], hgn4[:].rearrange("p h d -> p (h d)"), ALU.mult, [("sgg", s2), "hgn4"],
           [("gw", i_gw)])
        TT("pool", f_[s2], sg_[i_sg], omlbc[:], ALU.mult, [("sg", i_sg), "omlbc"], [("f", s2)])
        TT("pool", f_[s2], f_[s2], lbbc[:], ALU.add, [("f", s2), "lbbc"], [("f", s2)])
        yield
        ACT(logf_[s2], f_[s2], AF.Ln, [("f", s2)], [("logf", s2)])
        TS("pool", kk_[i_kk], f_[s2], -1.0, 1.0, ALU.mult, ALU.add, [("f", s2)], [("kk", i_kk)])
        yield
        MM(bk(5), triL[:], logf_[s2], True, True, ["triL", ("logf", s2)], [("bank", 5)])
        for h in range(4):
            MM(banks[4][:, 2 * h:2 * h + 2], logf_[s2][:, h * 128:(h + 1) * 128], chk[:], True, True,
               [("logf", s2), "chk"], [("bank", 4)])
        ACT(ebx[s2], bk(5), AF.Exp, [("bank", 5)], [("ebx", s2)])
        ACT(enb[s2], bk(5), AF.Exp, [("bank", 5)], [("enb", s2)], scale=-1.0)
        ACT(dec_[i_dec], banks[4][:, 0:8], AF.Exp, [("bank", 4)], [("dec", i_dec)])
        yield
        TT("dve", qbm[s2], qf[i_qf], ebx[s2], ALU.mult, [("qf", i_qf), ("ebx", s2)], [("qbm", s2)])
        TT("pool", kbm[i_kb], kk_[i_kk], enb[s2], ALU.mult, [("kk", i_kk), ("enb", s2)], [("kbm", i_kb)])
        yield
        pq = bkbf(3, 8, 128)
        for h in range(4):
            TR(pq[:, h, :], qbm[s2][:, h * 128:(h + 1) * 128], idb[:], [("qbm", s2), "idb"], [("bank", 3)])
        for h in range(4):
            TR(pq[:, 4 + h, :], kbm[i_kb][:, h * 128:(h + 1) * 128], idb[:], [("kbm", i_kb), "idb"], [("bank", 3)])
        CP("act", qbT[s2][:], pq[:, 0:4, :], [("bank", 3)], [("qbT", s2)])
        CP("act", kbT[s2][:], pq[:, 4:8, :], [("bank", 3)], [("kbT", s2)])
        yield
        CP("pool", qbT0[s2][:, :, 0:64], qbT[s2][:, :, 0:64], [("qbT", s2)], [("qbT0", s2)])
        CP("pool", qbT1[s2][:, :, 64:128], qbT[s2][:, :, 64:128], [("qbT", s2)], [("qbT1", s2)])
        psc = bk(6).rearrange("p (h d) -> p h d", d=128)
        for h in range(4):
            MM(psc[:, h, :], kbT[s2][:, h, :], qbT[s2][:, h, :], True, True, [("kbT", s2), ("qbT", s2)], [("bank", 6)])
        TT("dve", scT[s2][:], psc, triL[:].unsqueeze(1).to_broadcast([128, 4, 128]), ALU.mult,
           [("bank", 6), "triL"], [("scT", s2)])
        yield "sub"
        po = psc
        pu = bk(7).rearrange("p (h d) -> p h d", d=128)
        for h in range(4):
            MM(po[:, h, :], scT[s2][:, h, :], vg_[i_vg][:, h * 128:(h + 1) * 128], h == 0, False,
               [("scT", s2), ("vg", i_vg)], [("bank", 6)], skip=True)
        for c in range(2):
            cur = curs[0]
            for h in range(4):
                MM(pu[:, h, :], kbm[i_kb][c * 64:(c + 1) * 64, h * 128:(h + 1) * 128],
                   vg_[i_vg][c * 64:(c + 1) * 64, h * 128:(h + 1) * 128], True, True,
                   [("kbm", i_kb), ("vg", i_vg)], [("bank", 7)])
            qc = qbT0[s2] if c == 0 else qbT1[s2]
            kqc = ("qbT0", s2) if c == 0 else ("qbT1", s2)
            for h in range(4):
                MM(po[:, h, :], qc[:, h, :], state_bf[cur][:, h, :], False, c == 1, [kqc, ("sbf", cur)],
                   [("bank", 6)], skip=True)
            yield "sub"
            TT("dve", stmp[:], state[:], pu, ALU.add, ["state", ("bank", 7)], ["stmp"])
            dbc = dec_[i_dec].rearrange("p (h c) -> p h c", c=2)[:, :, c:c + 1].to_broadcast([128, 4, 128])
            TT("dve", state[:], stmp[:], dbc, ALU.mult, ["stmp", ("dec", i_dec)], ["state"])
            curs[0] = 1 - cur
            yield "sub"
            CP("act", state_bf[1 - cur][:], state[:], ["state"], [("sbf", 1 - cur)])
            if c == 0:
                yield "sub"
        CP("act", osb[s2], bk(6), [("bank", 6)], [("osb", s2)])
        yield "late"
        ss4 = ssq4b[s2]
        for h in range(4):
            ACT(hjunk, osb[s2][:, h * 128:(h + 1) * 128], AF.Square, [("osb", s2), ("ss4", s2)], ["hjunk", ("ss4", s2)],
                accum_out=ss4[:, h:h + 1])
        rstd_from_ssq(ss4[:], ("ss4", s2), ss4[:], ("ss4", s2), 128)
        yield "sub"
        for h in range(4):
            STT("dve", mixed[:, to, 512 + h * 128:512 + (h + 1) * 128], osb[s2][:, h * 128:(h + 1) * 128],
                ss4[:, h:h + 1], gw[i_gw][:, h * 128:(h + 1) * 128], ALU.mult, ALU.mult,
                [("osb", s2), ("ss4", s2), ("gw", i_gw)], [("mixedh", to)])
        MEMSET("pool", ss4[:], 0.0, [("ss4", s2)])

    run_pipeline(tileB2, NTO, prio=[9, 4, 3, 8, 6, 2, 7, 5, 1, 0])
    if debug:
        DBG("mixed_all", mixed, [128, NTO, 1024], [("mixedh", i) for i in range(NTO)])
    S.flush()
    if stage <= 3:
        return nc, dbg_list

    y_acc = av(R0, [128, NTO, 1024])
    B = Bump(R2, REND)
    wo_bf = B([128, 8, 1024], BF16)
    wo_st = [B([128, 4, 1024]) for _ in range(1)]
    xt = [B([128, 1024]) for _ in range(4)]
    mT = [B([128, 8, 128], BF16) for _ in range(3)]
    for half in range(2):
        DMA("sp", wo_st[0], woutd[half * 512:(half + 1) * 512, :].rearrange("(k q) n -> q k n", q=128),
            [], ["wo_st"], "wo_st")
        for j in range(4):
            eng = "dve" if j % 2 == 0 else "pool"
            TT(eng, wo_bf[:, half * 4 + j, :], wo_st[0][:, j, :], gate1bc[:], ALU.mult, ["wo_st"],
               [("wo", half * 4 + j)])
    def tileC1(to):
        sl = to % 3
        sx = to % 4
        t = NTC + to
        DMA("sp", xt[sx], xs[t * 128:(t + 1) * 128, :], [], [("xt", sx)], ("x", sx))
        yield
        pm = bkbf(to % 2, 8, 128)
        for j in range(8):
            TR(pm[:, j, :], mixed[:, to, j * 128:(j + 1) * 128], idb[:], ["idb"], [("bank", to % 2)])
        CP("act", mT[sl][:], pm, [("bank", to % 2)], [("mT", sl)])
        yield
        yield
        for half in range(2):
            bi = 2 + (to % 2) * 2 + half
            for j in range(8):
                MM(bk(bi), mT[sl][:, j, :], wo_bf[:, j, half * 512:(half + 1) * 512], j == 0, j == 7,
                   [("mT", sl), ("wo", j)], [("bank", bi)])
            TT("dve", y_acc[:, to, half * 512:(half + 1) * 512], bk(bi), xt[sx][:, half * 512:(half + 1) * 512],
               ALU.add, [("bank", bi), ("xt", sx)], [("y", to)])

    run_pipeline(tileC1, NTO)
    if debug:
        DBG("x1", y_acc, [128, NTO, 1024], [("y", i) for i in range(NTO)])
    S.flush()
    if stage <= 4:
        return nc, dbg_list

    h2T = av(R2, [128, 8, 2048], BF16)
    B = Bump(R1, R2)
    xn2 = [B([128, 1024]) for _ in range(2)]
    h2f = [B([128, 8, 128]) for _ in range(3)]
    wrt = B([128, 8, 36])
    junk2 = B([128, 1024], BF16)
    lgG = B([128, NTO, 4])
    lgE = B([128, NTO, 32])
    pen = B([128, NTO, 4])
    em = B([128, NTO, 32])
    oh = B([128, NTO, 32])
    r16 = [B([128, NTO]) for _ in range(8)]
    DMA("sp", wrt, wrtd.rearrange("(k q) n -> q k n", q=128), [], ["wrt"], "wrt")
    def tileC2(to):
        sl = to % 2
        sh = to % 3
        ssq, kss = new_ssq()
        ACT(junk2, y_acc[:, to, :], AF.Square, [kss], ["junk2", kss], accum_out=ssq)
        rstd_from_ssq(ssq, kss, ssq, kss, 1024)
        ACT(xn2[sl], y_acc[:, to, :], AF.Copy, [kss], [("xn2", sl)], scale=ssq)
        yield
        for j in range(8):
            bi = (to % 2) * 2 + j // 4
            TR(banks[bi][:, (j % 4) * 128:(j % 4 + 1) * 128], xn2[sl][:, j * 128:(j + 1) * 128], idf[:],
               [("xn2", sl), "idf"], [("bank", bi)])
        for j in range(8):
            bi = (to % 2) * 2 + j // 4
            TS("dve", h2f[sh][:, j, :], banks[bi][:, (j % 4) * 128:(j % 4 + 1) * 128], A2[:, j:j + 1], B2[:, j:j + 1],
               ALU.mult, ALU.add, [("bank", bi)], [("h2f", sh)])
        CP("pool", h2T[:, :, to * 128:(to + 1) * 128], h2f[sh][:], [("h2f", sh)], [("h2T", to // 4)])
        yield
        yield
        lb_ = 4 + to % 2
        for j in range(8):
            MM(banks[lb_][:, 0:36], h2f[sh][:, j, :], wrt[:, j, :], j == 0, j == 7, [("h2f", sh), "wrt"],
               [("bank", lb_)])
        TT("dve", lgG[:, to, :], banks[lb_][:, 0:4], rbbc[:, 0:4], ALU.add, [("bank", lb_)], ["lgG"])
        TT("dve", lgE[:, to, :], banks[lb_][:, 4:36], rbbc[:, 4:36], ALU.add, [("bank", lb_)], ["lgE"])

    run_pipeline(tileC2, NTO)
    gmax, gsum, gwt, m1, m2, dlt, coef, tmp16 = r16

    def bc(ap16, n):
        return ap16.unsqueeze(2).to_broadcast([128, NTO, n])

    RED("dve", gmax, lgG, ALU.max, ["lgG"], ["gmax"])
    TT("dve", pen, lgG, bc(gmax, 4), ALU.is_equal, ["lgG", "gmax"], ["pen"])
    TT("dve", lgG, lgG, bc(gmax, 4), ALU.subtract, ["lgG", "gmax", "pen"], ["lgG"])
    ACT(lgG, lgG, AF.Exp, ["lgG"], ["lgG"])
    RED("dve", gsum, lgG, ALU.add, ["lgG"], ["gsum"])
    RECIP(gwt, gsum, ["gsum"], ["gwt"])
    TS("dve", pen, pen, 1e30, -1e30, ALU.mult, ALU.add, ["pen"], ["pen"])
    TT("dve", em.rearrange("p t (g e) -> p (t g) e", e=8), lgE.rearrange("p t (g e) -> p (t g) e", e=8),
       pen.rearrange("p t g -> p (t g)").unsqueeze(2).to_broadcast([128, NTO * 4, 8]), ALU.add, ["lgE", "pen"], ["em"])
    RED("dve", m1, em, ALU.max, ["em"], ["m1"])
    TT("dve", oh, em, bc(m1, 32), ALU.is_equal, ["em", "m1"], ["oh"])
    STT("dve", oh, oh, -1e30, em, ALU.mult, ALU.add, ["oh", "em"], ["oh"])
    RED("dve", m2, oh, ALU.max, ["oh"], ["m2"])
    TT("dve", oh, em, bc(m2, 32), ALU.is_ge, ["em", "m2", "oh"], ["oh"])
    TT("dve", em, em, bc(m1, 32), ALU.subtract, ["em", "m1", "oh"], ["em"])
    ACT(em, em, AF.Exp, ["em"], ["em"])
    TT("dve", dlt, m2, m1, ALU.subtract, ["m1", "m2"], ["dlt"])
    ACT(dlt, dlt, AF.Exp, ["dlt"], ["dlt"])
    TS("dve", dlt, dlt, 1.0, None, ALU.add, None, ["dlt"], ["dlt"])
    RECIP(coef, dlt, ["dlt"], ["coef"])
    TT("dve", coef, coef, gwt, ALU.mult, ["coef", "gwt"], ["coef"])
    TT("dve", em, em, bc(coef, 32), ALU.mult, ["em", "coef"], ["em"])
    TT("dve", cb[:], em, oh, ALU.mult, ["em", "oh"], ["cb"])
    if debug:
        DBG("cb", cb[:], [128, NTO, 32], ["cb"])
    S.flush()
    if stage <= 5:
        return nc, dbg_list

    B = Bump(R1, R2)
    wgu = [B([128, 8, 512], BF16) for _ in range(2)]
    actT = [B([128, 2, 512], BF16) for _ in range(2)]
    sgl = [B([128, 512], BF16) for _ in range(2)]
    fnwbc = B([128, 1024])
    B = Bump(R3, REND)
    wd_st = [B([128, 2, 1024]) for _ in range(2)]
    wd_bf = [B([128, 2, 1024], BF16) for _ in range(2)]
    ojunk = B([128, 1024], BF16)
    DMA("sp", fnwbc, fnwd.partition_broadcast(128), [], ["fnw"], "fnw")

    def load_expert(e):
        sl = e % 2
        DMA("pool", wgu[sl][:, :, 0:256], wgd[e].rearrange("(k q) f -> q k f", q=128), [], [("wg", sl)], ("wg", sl))
        DMA("pool", wgu[sl][:, :, 256:512], wud[e].rearrange("(k q) f -> q k f", q=128), [], [("wu", sl)], ("wu", sl))
        DMA("sp", wd_st[sl], wdd[e].rearrange("(c q) d -> q c d", q=128), [], [("wds", sl)], ("wds", sl))
        for c in range(2):
            TT("pool", wd_bf[sl][:, c, :], wd_st[sl][:, c, :], gate2bc[:], ALU.mult, [("wds", sl)], [("wd", sl)])

    def gate_up(e, tb):
        sl = e % 2
        asl = tb % 2
        for fc in [0, 2, 1, 3]:
            for kc in range(8):
                MM(bk(fc), wgu[sl][:, kc, fc * 128:(fc + 1) * 128], h2T[:, kc, tb * 512:(tb + 1) * 512], kc == 0, kc == 7,
                   [("wg", sl), ("wu", sl), ("h2T", tb)], [("bank", fc)])
        for fc in range(2):
            ACT(sgl[fc], bk(fc), AF.Silu, [("bank", fc)], [("sgl", fc)])
            TT("dve", actT[asl][:, fc, :], sgl[fc], bk(2 + fc), ALU.mult, [("sgl", fc), ("bank", 2 + fc)],
               [("actT", asl)])

    def down(e, tb):
        sl = e % 2
        asl = tb % 2
        for ti in range(4):
            to = tb * 4 + ti
            for half in range(2):
                bi = 4 + (ti * 2 + half) % 4
                for fc in range(2):
                    MM(bk(bi), actT[asl][:, fc, ti * 128:(ti + 1) * 128], wd_bf[sl][:, fc, half * 512:(half + 1) * 512],
                       fc == 0, fc == 1, [("actT", asl), ("wd", sl)], [("bank", bi)])
                STT("dve", y_acc[:, to, half * 512:(half + 1) * 512], bk(bi), cb[:, to, e:e + 1],
                    y_acc[:, to, half * 512:(half + 1) * 512], ALU.mult, ALU.add, [("bank", bi), "cb"], [("y", to)])

    load_expert(0)
    pending = None
    n_exp = N_EXP
    for e in range(n_exp):
        for tb in range(4):
            gate_up(e, tb)
            if pending is not None:
                down(*pending)
            pending = (e, tb)
            if tb == 0 and e + 1 < n_exp:
                load_expert(e + 1)
    down(*pending)
    for to in range(NTO):
        ssq, kss = new_ssq()
        ACT(ojunk, y_acc[:, to, :], AF.Square, [("y", to), kss], ["ojunk", kss], accum_out=ssq)
        rstd_from_ssq(ssq, kss, ssq, kss, 1024)
        STT("dve", y_acc[:, to, :], y_acc[:, to, :], ssq, fnwbc, ALU.mult, ALU.mult, [("y", to), kss, "fnw"],
            [("y", to)])
        DMA("sp", outd[to * 128:(to + 1) * 128, :], y_acc[:, to, :], [("y", to)], [], ("out", to % 4))
    S.flush()
    return nc, dbg_list


def _consts():
    s = np.arange(128)
    same = (s[:, None] // 64) == (s[None, :] // 64)
    triL = (same & (s[:, None] <= s[None, :])).astype(np.float32)
    triU = (same & (s[:, None] > s[None, :])).astype(np.float32)
    chunkind = ((s[:, None] // 64) == np.arange(2)[None, :]).astype(np.float32)
    k = np.arange(128)[:, None, None]
    j = np.arange(4)[None, :, None]
    q = np.arange(512)[None, None, :]
    maskb = np.where(j * 128 + k <= q, 0.0, NEG).astype(np.float32).astype(ml_dtypes.bfloat16)
    half = 8
    inv_freq = (500000.0 ** (-np.arange(half, dtype=np.float32) / half)).astype(np.float32)
    invf = np.broadcast_to((inv_freq / np.float32(2 * np.pi)).astype(np.float32)[None, :], (128, 8)).copy()
    return dict(idb=np.eye(128, dtype=np.float32).astype(ml_dtypes.bfloat16), idf=np.eye(128, dtype=np.float32),
                triL=triL, triU=triU, chunkind=chunkind, maskb=maskb, invf=invf)


def col8(v):
    return np.ascontiguousarray(np.asarray(v, dtype=np.float32).reshape(8, 128).T)


def make_in_maps(inp):
    x = np.asarray(inp["x"], dtype=np.float32)
    pos = np.asarray(inp["positions"]).astype(np.int32)
    cst = _consts()
    shared = dict(
        n1col=col8(inp["norm1_w"][0]), n2col=col8(inp["norm2_w"][0]),
        adab=np.ascontiguousarray(np.asarray(inp["ada_b"], np.float32)[0][None, :]),
        adaw=np.ascontiguousarray(np.asarray(inp["ada_w"], np.float32)[0]),
        w_in=np.ascontiguousarray(np.asarray(inp["w_in"], np.float32)[0]),
        w_out=np.ascontiguousarray(np.asarray(inp["w_out"], np.float32)[0]),
        lamv=np.concatenate([np.asarray(inp[k], np.float32)[0] for k in
                             ("da_lambda_q1", "da_lambda_k1", "da_lambda_q2", "da_lambda_k2")])[None, :].copy(),
        subln=np.ascontiguousarray(np.asarray(inp["da_subln_w"], np.float32)[0][None, :]),
        hlb=np.ascontiguousarray(np.asarray(inp["hg_lower_bound"], np.float32).reshape(1, 1024)),
        hgnw=np.ascontiguousarray(np.asarray(inp["hg_norm_w"], np.float32)[0][None, :]),
        wrt=np.ascontiguousarray(np.concatenate([np.asarray(inp["moe_w_group"], np.float32)[0],
                                                 np.asarray(inp["moe_w_router"], np.float32)[0]], axis=1)),
        brt=np.ascontiguousarray(np.concatenate([np.asarray(inp["moe_b_group"], np.float32)[0],
                                                 np.asarray(inp["moe_b_router"], np.float32)[0]])[None, :]),
        w_gate=np.ascontiguousarray(np.asarray(inp["moe_w_gate"], np.float32)[0]),
        w_up=np.ascontiguousarray(np.asarray(inp["moe_w_up"], np.float32)[0]),
        w_down=np.ascontiguousarray(np.asarray(inp["moe_w_down"], np.float32)[0]),
        fnw=np.ascontiguousarray(np.asarray(inp["final_norm_w"], np.float32)[None, :]),
        **cst,
    )
    maps = []
    for i in range(8):
        b, half = i // 2, i % 2
        if half == 0:
            xs_ = np.concatenate([x[b, 0:2048], x[b, 0:2048]], axis=0)
            ps_ = np.concatenate([pos[b, 0:2048], pos[b, 0:2048]])
            ctx = np.tile(np.array([[NEG, 0.0]], np.float32), (128, 1))
        else:
            xs_ = x[b]
            ps_ = pos[b]
            ctx = np.tile(np.array([[0.0, 1.0]], np.float32), (128, 1))
        m = dict(shared)
        m["xs"] = np.ascontiguousarray(xs_)
        m["pos"] = np.ascontiguousarray(ps_.reshape(NT, 128).T)
        m["ctx"] = ctx
        m["ccol"] = col8(np.asarray(inp["c"], np.float32)[b])
        maps.append(m)
    return maps


_CACHE = {}


def kernel(**inputs):
    maps = make_in_maps(inputs)
    if "nc" not in _CACHE:
        _CACHE["nc"] = build_program()[0]
    nc = _CACHE["nc"]
    res = run_bass_kernel_spmd(nc, maps, core_ids=list(range(8)))
    out = np.empty((4, 4096, 1024), np.float32)
    for i in range(8):
        b, half = i // 2, i % 2
        out[b, half * 2048:(half + 1) * 2048] = res.results[i]["out"]
    return out
```
